# Optimizing a Trainium2 kernel written in Bass

```python
import math
import jax, jax.numpy as jnp
from jax import lax
import numpy as np

D_MODEL = 4096
BATCH = 2
SEQ = 4096
DEPTH = 4

MIX_WIDTH = D_MODEL
GROUP_WIDTH = MIX_WIDTH // 4
D_FF = (3 * D_MODEL) // 2
NORM_EPS = 1e-6
Q_BLOCK = 128

MLA_HEADS = GROUP_WIDTH // 128
MLA_Q_LORA = D_MODEL // 4
MLA_KV_LORA = D_MODEL // 8
MLA_NOPE = 128
MLA_ROPE = 64
MLA_V = GROUP_WIDTH // MLA_HEADS
ROPE_THETA = 10000.0

MLSTM_HEADS = 4
MLSTM_V = GROUP_WIDTH // MLSTM_HEADS
MLSTM_QK = MLSTM_V // 2
MLSTM_CONV = 5
MLSTM_CHUNK = 64

DIFF_HEADS = 8
DIFF_HEAD_DIM = GROUP_WIDTH // (2 * DIFF_HEADS)

SWA_HEADS = 16
SWA_KV_HEADS = 2
SWA_HEAD_DIM = GROUP_WIDTH // SWA_HEADS
SWA_WINDOW = 128

MLA_COLS = MLA_Q_LORA + MLA_KV_LORA + MLA_ROPE
MLSTM_COLS = 2 * MLSTM_HEADS * MLSTM_QK + 2 * MLSTM_HEADS * MLSTM_V + 4 * MLSTM_HEADS
DIFF_COLS = 3 * DIFF_HEADS * 2 * DIFF_HEAD_DIM
SWA_COLS = (SWA_HEADS + 2 * SWA_KV_HEADS) * SWA_HEAD_DIM
IN_COLS = MLA_COLS + MLSTM_COLS + DIFF_COLS + SWA_COLS

kernel_name = "hymba_style_hybrid_encoder"


def rms_norm(x, g):
    xf = x.astype(jnp.float32)
    y = xf * lax.rsqrt(jnp.mean(xf * xf, axis=-1, keepdims=True) + NORM_EPS)
    return (y * g.astype(jnp.float32)).astype(x.dtype)


def swiglu(h, w_gu, w_down):
    gate, up = jnp.split(h @ w_gu, 2, axis=-1)
    return (jax.nn.silu(gate) * up) @ w_down


def alibi_slopes(n):
    return 2.0 ** (-8.0 * jnp.arange(1, n + 1, dtype=jnp.float32) / n)


def rope(x, pos):
    half = x.shape[-1] // 2
    inv = ROPE_THETA ** (-jnp.arange(half, dtype=jnp.float32) / half)
    ang = pos.astype(jnp.float32)[:, None] * inv[None, :]
    cos = jnp.cos(ang)[None, :, None, :]
    sin = jnp.sin(ang)[None, :, None, :]
    xf = x.astype(jnp.float32)
    x1, x2 = xf[..., :half], xf[..., half:]
    return jnp.concatenate([x1 * cos - x2 * sin, x2 * cos + x1 * sin], axis=-1).astype(x.dtype)


def query_blocks(t):
    b, s = t.shape[:2]
    return jnp.moveaxis(t.reshape(b, s // Q_BLOCK, Q_BLOCK, *t.shape[2:]), 1, 0)


def merge_blocks(t):
    t = jnp.moveaxis(t, 0, 1)
    return t.reshape(t.shape[0], -1, *t.shape[3:])


def mla_mixer(z, q_norm, kv_norm, w_uq, w_ukv):
    b, s, _ = z.shape
    c_q, c_kv, k_pe = jnp.split(z, [MLA_Q_LORA, MLA_Q_LORA + MLA_KV_LORA], axis=-1)
    pos = jnp.arange(s)
    q = (rms_norm(c_q, q_norm) @ w_uq).reshape(b, s, MLA_HEADS, MLA_NOPE + MLA_ROPE)
    kv = (rms_norm(c_kv, kv_norm) @ w_ukv).reshape(b, s, MLA_HEADS, MLA_NOPE + MLA_V)
    k_nope, v = kv[..., :MLA_NOPE], kv[..., MLA_NOPE:]
    q = jnp.concatenate([q[..., :MLA_NOPE], rope(q[..., MLA_NOPE:], pos)], axis=-1)
    k_pe = rope(k_pe[:, :, None, :], pos)
    k = jnp.concatenate([k_nope, jnp.broadcast_to(k_pe, (b, s, MLA_HEADS, MLA_ROPE))], axis=-1)
    scale = (MLA_NOPE + MLA_ROPE) ** -0.5

    def block(qb):
        sc = jnp.einsum('bqhd,bkhd->bhqk', qb, k).astype(jnp.float32) * scale
        p = jax.nn.softmax(sc, axis=-1).astype(v.dtype)
        return jnp.einsum('bhqk,bkhd->bqhd', p, v)

    o = merge_blocks(lax.map(block, query_blocks(q)))
    return o.reshape(b, s, MLA_HEADS * MLA_V)


def centred_depthwise_conv(x, w, bias):
    c = x.shape[-1]
    pad = w.shape[0] // 2
    y = lax.conv_general_dilated(x, w[:, None, :].astype(x.dtype), window_strides=(1,),
                                 padding=((pad, pad),), dimension_numbers=('NWC', 'WIO', 'NWC'),
                                 feature_group_count=c)
    return y + bias.astype(x.dtype)


def mlstm_chunkwise(q, k, v, li, lf):
    b, h, s, dk = q.shape
    dv = v.shape[-1]
    L = MLSTM_CHUNK
    nc = s // L

    def chunks(t):
        return jnp.moveaxis(t.reshape(b, h, nc, L, *t.shape[3:]), 2, 0)

    qc, kc, vc, lic, lfc = chunks(q), chunks(k), chunks(v), chunks(li), chunks(lf)
    bc = jnp.cumsum(lfc, axis=-1)
    tri = jnp.tril(jnp.ones((L, L), dtype=bool))

    def step(carry, inp):
        C, n, m = carry
        qq, kk, vv, ii, bb = inp
        d_mat = jnp.where(tri, bb[..., :, None] - bb[..., None, :] + ii[..., None, :], -jnp.inf)
        m_inter = bb + m[..., None]
        m_t = jnp.maximum(m_inter, jnp.max(d_mat, axis=-1))
        w = jnp.exp(d_mat - m_t[..., None]) * jnp.einsum('bhtd,bhsd->bhts', qq, kk)
        inter = jnp.exp(m_inter - m_t)
        num = inter[..., None] * jnp.einsum('bhvd,bhtd->bhtv', C, qq) + jnp.einsum('bhts,bhsv->bhtv', w, vv)
        den = inter * jnp.einsum('bhd,bhtd->bht', n, qq) + jnp.sum(w, axis=-1)
        h_out = num / jnp.maximum(jnp.abs(den), jnp.exp(-m_t))[..., None]
        b_last = bb[..., -1]
        g = b_last[..., None] - bb + ii
        m_new = jnp.maximum(b_last + m, jnp.max(g, axis=-1))
        decay = jnp.exp(b_last + m - m_new)
        wgt = jnp.exp(g - m_new[..., None])
        C_new = decay[..., None, None] * C + jnp.einsum('bhsv,bhsd->bhvd', wgt[..., None] * vv, kk)
        n_new = decay[..., None] * n + jnp.einsum('bhs,bhsd->bhd', wgt, kk)
        return (C_new, n_new, m_new), h_out

    init = (jnp.zeros((b, h, dv, dk), jnp.float32), jnp.zeros((b, h, dk), jnp.float32),
            jnp.full((b, h), -jnp.inf, jnp.float32))
    _, hs = lax.scan(step, init, (qc, kc, vc, lic, bc))
    return jnp.moveaxis(hs, 0, 2).reshape(b, h, s, dv)


def mlstm_mixer(z, conv_w, conv_b, gate_b):
    b, s, _ = z.shape
    qk_w = 2 * MLSTM_HEADS * MLSTM_QK
    v_w = MLSTM_HEADS * MLSTM_V
    qk, v, o, gates = jnp.split(z, [qk_w, qk_w + v_w, qk_w + 2 * v_w], axis=-1)
    qk = jax.nn.silu(centred_depthwise_conv(qk, conv_w, conv_b))
    q, k = jnp.split(qk, 2, axis=-1)
    heads = lambda t, d: t.astype(jnp.float32).reshape(b, s, MLSTM_HEADS, d).transpose(0, 2, 1, 3)
    q = heads(q, MLSTM_QK)
    k = heads(k, MLSTM_QK) * (MLSTM_QK ** -0.5)
    v = heads(v, MLSTM_V)
    g = (gates.astype(jnp.float32).reshape(b, s, 4, MLSTM_HEADS) + gate_b.astype(jnp.float32)).transpose(0, 2, 3, 1)
    li_f, lf_f = g[:, 0], jax.nn.log_sigmoid(g[:, 1])
    li_b, lf_b = g[:, 2], jax.nn.log_sigmoid(g[:, 3])
    flip = lambda t: jnp.flip(t, axis=2)
    h_f = mlstm_chunkwise(q, k, v, li_f, lf_f)
    h_b = flip(mlstm_chunkwise(flip(q), flip(k), flip(v), flip(li_b), flip(lf_b)))
    h = (h_f + h_b).transpose(0, 2, 1, 3).reshape(b, s, MLSTM_HEADS * MLSTM_V)
    return (jax.nn.sigmoid(o.astype(jnp.float32)) * h).astype(z.dtype)


def diff_mixer(z, lam_vecs, subln, lam_init):
    b, s, _ = z.shape
    q, k, v = jnp.split(z, 3, axis=-1)
    q = q.reshape(b, s, DIFF_HEADS, 2, DIFF_HEAD_DIM)
    k = k.reshape(b, s, DIFF_HEADS, 2, DIFF_HEAD_DIM)
    v = v.reshape(b, s, DIFF_HEADS, 2 * DIFF_HEAD_DIM)
    k1, k2 = k[..., 0, :], k[..., 1, :]
    lv = lam_vecs.astype(jnp.float32)
    lam = jnp.exp(jnp.sum(lv[0] * lv[1])) - jnp.exp(jnp.sum(lv[2] * lv[3])) + lam_init
    slopes = alibi_slopes(DIFF_HEADS)
    kpos = jnp.arange(s)
    scale = DIFF_HEAD_DIM ** -0.5

    def block(args):
        q1b, q2b, start = args
        qpos = start + jnp.arange(Q_BLOCK)
        bias = -slopes[:, None, None] * jnp.abs(qpos[:, None] - kpos[None, :]).astype(jnp.float32)
        s1 = jnp.einsum('bqhd,bkhd->bhqk', q1b, k1).astype(jnp.float32) * scale + bias
        s2 = jnp.einsum('bqhd,bkhd->bhqk', q2b, k2).astype(jnp.float32) * scale + bias
        p = jax.nn.softmax(s1, axis=-1) - lam * jax.nn.softmax(s2, axis=-1)
        return jnp.einsum('bhqk,bkhd->bqhd', p.astype(v.dtype), v)

    starts = jnp.arange(s // Q_BLOCK, dtype=jnp.int32) * Q_BLOCK
    o = merge_blocks(lax.map(block, (query_blocks(q[..., 0, :]), query_blocks(q[..., 1, :]), starts)))
    o = rms_norm(o, subln) * (1.0 - lam_init)
    return o.reshape(b, s, DIFF_HEADS * 2 * DIFF_HEAD_DIM)


def swa_mixer(z, sink):
    b, s, _ = z.shape
    W = SWA_WINDOW
    nb = s // W
    rep = SWA_HEADS // SWA_KV_HEADS
    q_w = SWA_HEADS * SWA_HEAD_DIM
    kv_w = SWA_KV_HEADS * SWA_HEAD_DIM
    q, k, v = jnp.split(z, [q_w, q_w + kv_w], axis=-1)
    q = q.reshape(b, nb, W, SWA_KV_HEADS, rep, SWA_HEAD_DIM)

    def bands(t):
        t = t.reshape(b, s, SWA_KV_HEADS, SWA_HEAD_DIM)
        tp = jnp.pad(t, ((0, 0), (W, W), (0, 0), (0, 0))).reshape(b, nb + 2, W, SWA_KV_HEADS, SWA_HEAD_DIM)
        return jnp.concatenate([tp[:, :-2], tp[:, 1:-1], tp[:, 2:]], axis=2)

    kb, vb = bands(k), bands(v)
    qpos = jnp.arange(nb)[:, None] * W + jnp.arange(W)[None, :]
    kpos = (jnp.arange(nb)[:, None] - 1) * W + jnp.arange(3 * W)[None, :]
    rel = jnp.abs(qpos[:, :, None] - kpos[:, None, :])
    valid = (rel <= W) & (kpos >= 0)[:, None, :] & (kpos < s)[:, None, :]
    slopes = alibi_slopes(SWA_HEADS).reshape(SWA_KV_HEADS, rep)
    bias = jnp.where(valid[None, :, None, None],
                     -slopes[None, None, :, :, None, None] * rel[None, :, None, None].astype(jnp.float32),
                     -jnp.inf)
    sc = jnp.einsum('bnqgrd,bnkgd->bngrqk', q, kb).astype(jnp.float32) * (SWA_HEAD_DIM ** -0.5) + bias
    sink_l = jnp.broadcast_to(sink.astype(jnp.float32).reshape(SWA_KV_HEADS, rep)[None, None, :, :, None, None],
                              sc.shape[:-1] + (1,))
    p = jax.nn.softmax(jnp.concatenate([sc, sink_l], axis=-1), axis=-1)[..., :-1]
    o = jnp.einsum('bngrqk,bnkgd->bnqgrd', p.astype(vb.dtype), vb)
    return o.reshape(b, s, SWA_HEADS * SWA_HEAD_DIM)


def setup_inputs(seed: int = 0) -> dict:
    key = jax.random.key(seed)
    ks = jax.random.split(key, 20)
    f32 = jnp.float32
    nrm = lambda k, shape: jax.random.normal(k, shape, f32)
    dense = lambda k, shape, fan_in: nrm(k, shape) * (fan_in ** -0.5)
    gain = lambda k, shape: 1.0 + 0.05 * nrm(k, shape)
    forget_base = jnp.linspace(3.0, 6.0, MLSTM_HEADS, dtype=f32)
    zero_base = jnp.zeros((MLSTM_HEADS,), f32)
    gate_base = jnp.stack([zero_base, forget_base, zero_base, forget_base])
    return {
        "x": nrm(ks[0], (BATCH, SEQ, D_MODEL)),
        "norm_gains": gain(ks[1], (DEPTH, 6, D_MODEL)),
        "w_in": dense(ks[2], (DEPTH, D_MODEL, IN_COLS), D_MODEL),
        "mla_q_norm": gain(ks[3], (DEPTH, MLA_Q_LORA)),
        "mla_kv_norm": gain(ks[4], (DEPTH, MLA_KV_LORA)),
        "mla_w_uq": dense(ks[5], (DEPTH, MLA_Q_LORA, MLA_HEADS * (MLA_NOPE + MLA_ROPE)), MLA_Q_LORA),
        "mla_w_ukv": dense(ks[6], (DEPTH, MLA_KV_LORA, MLA_HEADS * (MLA_NOPE + MLA_V)), MLA_KV_LORA),
        "mlstm_conv_w": dense(ks[7], (DEPTH, MLSTM_CONV, 2 * MLSTM_HEADS * MLSTM_QK), MLSTM_CONV),
        "mlstm_conv_b": 0.01 * nrm(ks[8], (DEPTH, 2 * MLSTM_HEADS * MLSTM_QK)),
        "mlstm_gate_b": gate_base[None] + 0.1 * nrm(ks[9], (DEPTH, 4, MLSTM_HEADS)),
        "diff_lambda": 0.1 * nrm(ks[10], (DEPTH, 4, DIFF_HEAD_DIM)),
        "diff_subln": gain(ks[11], (DEPTH, 2 * DIFF_HEAD_DIM)),
        "swa_sink": 0.5 * nrm(ks[12], (DEPTH, SWA_HEADS)),
        "group_norm": gain(ks[13], (DEPTH, MIX_WIDTH)),
        "w_out": dense(ks[14], (DEPTH, MIX_WIDTH, D_MODEL), MIX_WIDTH),
        "ffn1_w_gu": dense(ks[15], (DEPTH, D_MODEL, 2 * D_FF), D_MODEL),
        "ffn1_w_down": dense(ks[16], (DEPTH, D_FF, D_MODEL), D_FF),
        "ffn2_w_gu": dense(ks[17], (DEPTH, D_MODEL, 2 * D_FF), D_MODEL),
        "ffn2_w_down": dense(ks[18], (DEPTH, D_FF, D_MODEL), D_FF),
    }


def reference(x, norm_gains, w_in, mla_q_norm, mla_kv_norm, mla_w_uq, mla_w_ukv, mlstm_conv_w,
              mlstm_conv_b, mlstm_gate_b, diff_lambda, diff_subln, swa_sink, group_norm, w_out,
              ffn1_w_gu, ffn1_w_down, ffn2_w_gu, ffn2_w_down):
    split_at = [MLA_COLS, MLA_COLS + MLSTM_COLS, MLA_COLS + MLSTM_COLS + DIFF_COLS]
    for l in range(DEPTH):
        g = norm_gains[l]
        h = swiglu(rms_norm(x, g[0]), ffn1_w_gu[l], ffn1_w_down[l])
        x = x + 0.5 * rms_norm(h, g[1])
        z = rms_norm(x, g[2]) @ w_in[l]
        z_a, z_b, z_c, z_d = jnp.split(z, split_at, axis=-1)
        lam_init = 0.8 - 0.6 * math.exp(-0.3 * l)
        y_a = mla_mixer(z_a, mla_q_norm[l], mla_kv_norm[l], mla_w_uq[l], mla_w_ukv[l])
        y_b = mlstm_mixer(z_b, mlstm_conv_w[l], mlstm_conv_b[l], mlstm_gate_b[l])
        y_c = diff_mixer(z_c, diff_lambda[l], diff_subln[l], lam_init)
        y_d = swa_mixer(z_d, swa_sink[l])
        gn = jnp.split(group_norm[l], 4)
        y = jnp.concatenate([rms_norm(y_a, gn[0]), rms_norm(y_b, gn[1]),
                             rms_norm(y_c, gn[2]), rms_norm(y_d, gn[3])], axis=-1) @ w_out[l]
        x = x + rms_norm(y, g[3])
        h = swiglu(rms_norm(x, g[4]), ffn2_w_gu[l], ffn2_w_down[l])
        x = x + 0.5 * rms_norm(h, g[5])
    return x
```

```python
import math
import numpy as np
from contextlib import ExitStack
import concourse.bass as bass
import concourse.mybir as mybir
from concourse.bass_utils import run_bass_kernel_spmd

F32 = mybir.dt.float32
BF16 = mybir.dt.bfloat16
AF = mybir.ActivationFunctionType
ALU = mybir.AluOpType

D = 4096
P = 128
KC = D // P
TT = 512
EPS = 1e-6
DFF = 6144
NDFF = DFF // P
IN_COLS = 9040
OFF_A, OFF_B, OFF_C, OFF_D = 0, 1600, 4688, 7760
NEG = -1.0e30
AHEAD = 3

COMPUTE = ("pe", "act", "dve", "pool")
ENGS = ("pe", "act", "dve", "pool", "sp")


class Rec:
    __slots__ = ("eng", "fn", "deps", "dma", "key", "kidx", "sig", "cnt")

    def __init__(self, eng, fn, deps, dma, key, kidx):
        self.eng = eng; self.fn = fn; self.deps = deps; self.dma = dma
        self.key = key; self.kidx = kidx; self.sig = False; self.cnt = 0


class Sched:
    def __init__(self, nc):
        self.nc = nc
        self.streams = {e: [] for e in ENGS}
        self.last_w = {}
        self.readers = {}
        self.dma_keys = {}
        self.last_by_key = {}
        self.pending = {}

    def op(self, eng, fn, reads=(), writes=(), dma_key=None):
        deps = []
        lw = self.last_w; rd = self.readers
        for r in reads:
            w = lw.get(r)
            if w is not None:
                deps.append(w)
        for r in writes:
            w = lw.get(r)
            if w is not None:
                deps.append(w)
            rs = rd.get(r)
            if rs:
                deps.extend(rs)
        pb = self.pending.pop(eng, None)
        if pb:
            deps.extend(pb)
        dma = dma_key is not None
        kidx = 0
        if dma:
            kidx = self.dma_keys.get(dma_key, 0)
            self.dma_keys[dma_key] = kidx + 1
        rec = Rec(eng, fn, deps, dma, dma_key, kidx)
        if dma:
            self.last_by_key[dma_key] = rec
        for d in deps:
            d.sig = True
        for r in writes:
            lw[r] = rec
            rd[r] = []
        for r in reads:
            l = rd.get(r)
            if l is None:
                rd[r] = [rec]
            else:
                l.append(rec)
        self.streams[eng].append(rec)
        return rec

    def barrier(self):
        recs = []
        for e in ENGS:
            for r in reversed(self.streams[e]):
                if not r.dma:
                    recs.append(r)
                    break
        recs += list(self.last_by_key.values())
        self.pending = {e: list(recs) for e in ENGS}
        self.last_w = {}
        self.readers = {}

    def emit(self):
        nc = self.nc
        with ExitStack() as es:
            esem = {e: es.enter_context(nc.semaphore("sem_" + e)) for e in COMPUTE}
            ksem = {}
            for i, k in enumerate(self.dma_keys):
                ksem[k] = es.enter_context(nc.semaphore("semk_%d" % i))
            for e in COMPUTE:
                c = 0
                for r in self.streams[e]:
                    if r.sig and not r.dma:
                        c += 1
                        r.cnt = c
            streams = self.streams

            def run(ename, eng):
                seen = {}
                for r in streams[ename]:
                    need = {}
                    for d in r.deps:
                        if d.dma:
                            s = ("k", d.key); v = 16 * (d.kidx + 1)
                        else:
                            if d.eng == ename and ename == "pe":
                                continue
                            s = ("e", d.eng); v = d.cnt
                        if seen.get(s, 0) >= v:
                            continue
                        if need.get(s, 0) < v:
                            need[s] = v
                    for s, v in need.items():
                        seen[s] = v
                        eng.wait_ge(ksem[s[1]] if s[0] == "k" else esem[s[1]], v)
                    ins = r.fn(eng)
                    if r.dma:
                        ins.then_inc(ksem[r.key], 16)
                    elif r.sig:
                        ins.then_inc(esem[ename], 1)
                if ename == "sp":
                    for k, n in self.dma_keys.items():
                        eng.wait_ge(ksem[k], 16 * n)

            with nc.Block() as block:
                @block.tensor
                def _(e): run("pe", e)

                @block.scalar
                def _(e): run("act", e)

                @block.vector
                def _(e): run("dve", e)

                @block.gpsimd
                def _(e): run("pool", e)

                @block.sync
                def _(e): run("sp", e)


class Ctx:
    pass


def rot(c, name, n):
    i = c.rot.get(name, 0)
    c.rot[name] = i + 1
    return i % n


class Bufs:
    def __init__(self, c, es, wslot_elems=4096, nw=4, xn=True, actt=False):
        nc = c.nc
        c.bufid += 1
        t = "b%d_" % c.bufid
        sb = lambda name, shape, dt: es.enter_context(nc.sbuf_tensor(t + name, shape, dt))
        self.sb = sb
        self.wslots = [sb("w%d" % i, [P, wslot_elems], BF16) for i in range(nw)]
        if xn:
            self.XN = sb("XN", [P, KC, TT], BF16)
        if actt:
            self.ACTT = sb("ACTT", [P, NDFF, TT], BF16)
        self.XS = [sb("XS%d" % i, [P, TT], F32) for i in range(4)]
        self.HS = [sb("HS%d" % i, [P, TT], F32) for i in range(3)]
        self.OS = [sb("OS%d" % i, [P, TT], F32) for i in range(3)]
        self.OB = [sb("OB%d" % i, [P, TT], BF16) for i in range(3)]
        self.SQ = [sb("SQ%d" % i, [P, TT], BF16) for i in range(3)]
        self.SG = [sb("SG%d" % i, [P, TT], F32) for i in range(3)]
        self.T1 = sb("T1", [P, TT], F32)
        self.R = [sb("R%d" % i, [P, TT], F32) for i in range(2)]


def cast_weight(c, name, w_in, w_bf, ng, row_elems):
    sch = c.sch
    bb = 2048
    while row_elems % bb:
        bb //= 2
    recs = []
    for g in range(ng):
        src = w_in[g].rearrange("p (a b) -> p a b", b=bb)
        dst = w_bf[g].rearrange("p (a b) -> p a b", b=bb)
        recs.append(sch.op("pool", (lambda e, s=src, d=dst: e.dma_start(out=d, in_=s)),
                           writes=[("wbf", name, g)], dma_key=("cast", name)))
    for r in recs:
        r.kidx = recs[-1].kidx


def load_w(c, b, name, w_bf, g, nkc, gw):
    i = rot(c, "w", len(b.wslots))
    slot = b.wslots[i]
    n = nkc * gw
    res = ("wslot", i)
    c.sch.op("sp", (lambda e, s=slot, src=w_bf[g], n=n: e.dma_start(out=s[:, 0:n], in_=src)),
             reads=[("wbf", name + c.wsuf, g)], writes=[res], dma_key=("wslot", i))
    return slot[:, 0:n].rearrange("p (k c) -> p k c", c=gw), res


class WPipe:
    def __init__(self, c, b, items):
        self.c = c; self.b = b; self.items = items; self.issued = []; self.pos = 0

    def next(self):
        ahead = len(self.b.wslots) - 1
        while len(self.issued) < min(len(self.items), self.pos + 1 + ahead):
            name, w_bf, g, nkc, gw = self.items[len(self.issued)]
            self.issued.append(load_w(self.c, self.b, name, w_bf, g, nkc, gw))
        r = self.issued[self.pos]
        self.pos += 1
        return r


def ld_tile(c, b, src_ap, rows=P, cols=TT, reads=()):
    i = rot(c, "XS", len(b.XS)); xs = b.XS[i]
    c.sch.op("sp", (lambda e: e.dma_start(out=xs[0:rows, 0:cols], in_=src_ap)),
             reads=list(reads), writes=[("XS", i)], dma_key=("XS", i))
    return xs, ("XS", i)


def rms_rstd(c, b, src_dram, t0, nchunks, ps_bank, ridx, res_fn=None):
    sch = c.sch
    ps = c.ps[ps_bank]
    for kc in range(nchunks):
        xs, rx = ld_tile(c, b, src_dram[kc * P:(kc + 1) * P, t0:t0 + TT], reads=([res_fn(kc)] if res_fn else ()))
        j = rot(c, "SQ", len(b.SQ)); sq = b.SQ[j]
        sch.op("act", (lambda e, xs=xs, sq=sq: e.activation(out=sq[:], in_=xs[:], func=AF.Square)),
               reads=[rx], writes=[("SQ", j)])
        sch.op("pe", (lambda e, sq=sq, kc=kc: e.matmul(ps[:], lhsT=c.ones_bf[:], rhs=sq[:], start=(kc == 0), stop=(kc == nchunks - 1))),
               reads=[("SQ", j), "ones_bf"], writes=[("ps", ps_bank)])
    nfeat = nchunks * P
    sch.op("act", (lambda e: e.activation(out=b.T1[:], in_=ps[:], func=AF.Sqrt, bias=c.epsc[:, 0:1], scale=1.0 / nfeat)),
           reads=[("ps", ps_bank)], writes=["T1"])
    sch.op("dve", (lambda e: e.reciprocal(out=b.R[ridx][:], in_=b.T1[:])), reads=["T1"], writes=[("R", ridx)])


def norm_to(c, b, src_dram, t0, nchunks, gain_ap_fn, ridx, dst_fn, dst_res_fn, res_fn=None):
    for kc in range(nchunks):
        xs, rx = ld_tile(c, b, src_dram[kc * P:(kc + 1) * P, t0:t0 + TT], reads=([res_fn(kc)] if res_fn else ()))
        c.sch.op("dve", (lambda e, xs=xs, kc=kc: e.scalar_tensor_tensor(
            out=dst_fn(kc), in0=xs[:], scalar=gain_ap_fn(kc), in1=b.R[ridx][:], op0=ALU.mult, op1=ALU.mult)),
            reads=[rx, ("R", ridx), "consts"], writes=[dst_res_fn(kc)])


def post_proj(c, b, wp, rhs_fn, rhs_res_fn, nk, gain_fn, factor, src_dram, dst_dram, hT, t0):
    sch = c.sch
    pend = None
    for m in range(KC):
        w, rw = wp.next()
        bk = 4 + (m % 2)
        for kc in range(nk):
            sch.op("pe", (lambda e, w=w, kc=kc, bk=bk: e.matmul(c.ps[bk][:], lhsT=w[:, kc, :], rhs=rhs_fn(kc), start=(kc == 0), stop=(kc == nk - 1))),
                   reads=[rw, rhs_res_fn(kc)], writes=[("ps", bk)])
        if pend is not None:
            pend()
        i = rot(c, "HS", len(b.HS)); hs = b.HS[i]
        sch.op("act", (lambda e, hs=hs, bk=bk: e.activation(out=hs[:], in_=c.ps[bk][:], func=AF.Copy)),
               reads=[("ps", bk)], writes=[("HS", i)])
        j = rot(c, "SQ", len(b.SQ)); sq = b.SQ[j]
        sch.op("act", (lambda e, sq=sq, bk=bk: e.activation(out=sq[:], in_=c.ps[bk][:], func=AF.Square)),
               reads=[("ps", bk)], writes=[("SQ", j)])
        sch.op("sp", (lambda e, hs=hs, m=m: e.dma_start(out=hT[m * P:(m + 1) * P, :], in_=hs[:])),
               reads=[("HS", i)], writes=[("hT", m)], dma_key=("HS", i))

        def mk(sq=sq, j=j, m=m):
            def f():
                sch.op("pe", (lambda e: e.matmul(c.ps[6][:], lhsT=c.ones_bf[:], rhs=sq[:], start=(m == 0), stop=(m == KC - 1))),
                       reads=[("SQ", j), "ones_bf"], writes=[("ps", 6)])
            return f
        pend = mk()
    pend()
    sch.op("act", (lambda e: e.activation(out=b.T1[:], in_=c.ps[6][:], func=AF.Sqrt, bias=c.epsc[:, 0:1], scale=1.0 / D)),
           reads=[("ps", 6)], writes=["T1"])
    sch.op("dve", (lambda e: e.reciprocal(out=b.R[1][:], in_=b.T1[:])), reads=["T1"], writes=[("R", 1)])
    for m in range(KC):
        i = rot(c, "HS", len(b.HS)); hs = b.HS[i]
        sch.op("sp", (lambda e, hs=hs, m=m: e.dma_start(out=hs[:], in_=hT[m * P:(m + 1) * P, :])),
               reads=[("hT", m)], writes=[("HS", i)], dma_key=("HS", i))
        xs, rx = ld_tile(c, b, src_dram[m * P:(m + 1) * P, t0:t0 + TT])
        k = rot(c, "SG", len(b.SG)); sg = b.SG[k]
        sch.op("dve", (lambda e, hs=hs, sg=sg, m=m: e.scalar_tensor_tensor(
            out=sg[:], in0=hs[:], scalar=gain_fn(m), in1=b.R[1][:], op0=ALU.mult, op1=ALU.mult)),
            reads=[("HS", i), ("R", 1), "consts"], writes=[("SG", k)])
        o = rot(c, "OS", len(b.OS)); os_ = b.OS[o]
        sch.op("dve", (lambda e, sg=sg, xs=xs, os_=os_: e.scalar_tensor_tensor(
            out=os_[:], in0=sg[:], scalar=float(factor), in1=xs[:], op0=ALU.mult, op1=ALU.add)),
            reads=[("SG", k), rx], writes=[("OS", o)])
        sch.op("sp", (lambda e, os_=os_, m=m: e.dma_start(out=dst_dram[m * P:(m + 1) * P, t0:t0 + TT], in_=os_[:])),
               reads=[("OS", o)], writes=[("act", id(dst_dram), m, t0)], dma_key=("OS", o))


def ffn_phase(c, src_dram, dst_dram, wgu, wd, gains, g_pre, g_post, wname):
    sch = c.sch
    with ExitStack() as es:
        b = Bufs(c, es, wslot_elems=NDFF * P, actt=True)
        for tt in range(c.S // TT):
            t0 = tt * TT
            rms_rstd(c, b, src_dram, t0, KC, 7, 0)
            norm_to(c, b, src_dram, t0, KC, lambda kc: gains[:, g_pre * KC + kc:g_pre * KC + kc + 1], 0,
                    lambda kc: b.XN[:, kc, :], lambda kc: ("XN", kc))
            items = []
            for j in range(NDFF):
                items.append((wname + "gu", wgu, j, KC, P)); items.append((wname + "gu", wgu, NDFF + j, KC, P))
            for m in range(KC):
                items.append((wname + "dn", wd, m, NDFF, P))
            wp = WPipe(c, b, items)
            for j in range(NDFF):
                bg = 2 * (j % 2); bu = bg + 1
                wg, rg = wp.next()
                for kc in range(KC):
                    sch.op("pe", (lambda e, wg=wg, kc=kc, bg=bg: e.matmul(c.ps[bg][:], lhsT=wg[:, kc, :], rhs=b.XN[:, kc, :], start=(kc == 0), stop=(kc == KC - 1))),
                           reads=[rg, ("XN", kc)], writes=[("ps", bg)])
                wu, ru = wp.next()
                for kc in range(KC):
                    sch.op("pe", (lambda e, wu=wu, kc=kc, bu=bu: e.matmul(c.ps[bu][:], lhsT=wu[:, kc, :], rhs=b.XN[:, kc, :], start=(kc == 0), stop=(kc == KC - 1))),
                           reads=[ru, ("XN", kc)], writes=[("ps", bu)])
                k = rot(c, "SG", len(b.SG)); sg = b.SG[k]
                sch.op("act", (lambda e, sg=sg, bg=bg: e.activation(out=sg[:], in_=c.ps[bg][:], func=AF.Silu)),
                       reads=[("ps", bg)], writes=[("SG", k)])
                sch.op("dve", (lambda e, sg=sg, bu=bu, j=j: e.tensor_tensor(out=b.ACTT[:, j, :], in0=sg[:], in1=c.ps[bu][:], op=ALU.mult)),
                       reads=[("SG", k), ("ps", bu)], writes=[("ACTT", j)])
            post_proj(c, b, wp, lambda kc: b.ACTT[:, kc, :], lambda kc: ("ACTT", kc), NDFF,
                      lambda m: gains[:, g_post * KC + m:g_post * KC + m + 1], 0.5, src_dram, dst_dram, c.hT, t0)
    sch.barrier()


def win_groups():
    g = []
    for i in range(8): g.append(("cq", i, OFF_A + i * 128, 128))
    for i in range(4): g.append(("ckv", i, OFF_A + 1024 + i * 128, 128))
    g.append(("kpe", 0, OFF_A + 1536, 32)); g.append(("kpe", 1, OFF_A + 1568, 32))
    for i in range(8): g.append(("mqk", i, OFF_B + i * 128, 128))
    for i in range(8): g.append(("mv", i, OFF_B + 1024 + i * 128, 128))
    for i in range(8): g.append(("mo", i, OFF_B + 2048 + i * 128, 128))
    g.append(("mg", 0, OFF_B + 3072, 16))
    for i in range(8): g.append(("dq", i, OFF_C + i * 128, 128))
    for i in range(8): g.append(("dk", i, OFF_C + 1024 + i * 128, 128))
    for i in range(8): g.append(("dv", i, OFF_C + 2048 + i * 128, 128))
    for i in range(8): g.append(("sq", i, OFF_D + i * 128, 128))
    g.append(("sk", 0, OFF_D + 1024, 128)); g.append(("sv", 0, OFF_D + 1152, 128))
    return g


TOKMAJ = ("mv", "dv", "sv")


def evac(c, b, bank, rows, func, dst_ap, bf, res, bias=None, cols=TT):
    sch = c.sch
    if bf:
        o = rot(c, "OB", len(b.OB)); st = b.OB[o]; r = ("OB", o)
    else:
        o = rot(c, "OS", len(b.OS)); st = b.OS[o]; r = ("OS", o)
    kw = {} if bias is None else {"bias": bias}
    sch.op("act", (lambda e: e.activation(out=st[0:rows, 0:cols], in_=c.ps[bank][0:rows, 0:cols], func=func, **kw)),
           reads=[("ps", bank), "consts"], writes=[r])
    sch.op("sp", (lambda e: e.dma_start(out=dst_ap, in_=st[0:rows, 0:cols])), reads=[r], writes=[res], dma_key=r)


def proj_fm(c, b, w, rw, nk, rhs_fn, rhs_res_fn, bank, M):
    for kc in range(nk):
        c.sch.op("pe", (lambda e, kc=kc: e.matmul(c.ps[bank][0:M, :], lhsT=w[:, kc, 0:M], rhs=rhs_fn(kc), start=(kc == 0), stop=(kc == nk - 1))),
                 reads=[rw, rhs_res_fn(kc)], writes=[("ps", bank)])


def proj_tm(c, b, w, rw, nk, xn_fn, xn_res_fn, bank):
    for tb in range(4):
        for kc in range(nk):
            c.sch.op("pe", (lambda e, kc=kc, tb=tb: e.matmul(c.ps[bank][:, tb * P:(tb + 1) * P], lhsT=xn_fn(kc)[:, tb * P:(tb + 1) * P], rhs=w[:, kc, :],
                                                            start=(kc == 0), stop=(kc == nk - 1))),
                     reads=[rw, xn_res_fn(kc)], writes=[("ps", bank)])


def store_tm(c, b, bank, vdram, t0, col0, res):
    sch = c.sch
    o = rot(c, "OB", len(b.OB)); st = b.OB[o]; r = ("OB", o)
    sch.op("act", (lambda e: e.activation(out=st[:], in_=c.ps[bank][:], func=AF.Copy)), reads=[("ps", bank)], writes=[r])
    dst = vdram[t0:t0 + TT, col0:col0 + P].rearrange("(tb p) c -> p tb c", p=P)
    sch.op("sp", (lambda e: e.dma_start(out=dst, in_=st[:].rearrange("p (tb c) -> p tb c", c=P))), reads=[r], writes=[res], dma_key=r)


def rope(c, b, x1, r1, x2, r2, cs, dst, row0, t0, res):
    sch = c.sch
    cos, sin = cs
    def tmp():
        k = rot(c, "SG", len(b.SG)); return b.SG[k], ("SG", k)
    ta, ra = tmp(); tb_, rb = tmp()
    sch.op("dve", (lambda e: e.tensor_tensor(out=ta[0:32, :], in0=x1, in1=cos, op=ALU.mult)), reads=[r1, "cs"], writes=[ra])
    sch.op("dve", (lambda e: e.tensor_tensor(out=tb_[0:32, :], in0=x2, in1=sin, op=ALU.mult)), reads=[r2, "cs"], writes=[rb])
    o = rot(c, "OB", len(b.OB)); st = b.OB[o]; ro = ("OB", o)
    sch.op("dve", (lambda e: e.tensor_tensor(out=st[0:32, :], in0=ta[0:32, :], in1=tb_[0:32, :], op=ALU.subtract)), reads=[ra, rb], writes=[ro])
    sch.op("sp", (lambda e: e.dma_start(out=dst[row0:row0 + 32, t0:t0 + TT], in_=st[0:32, :])), reads=[ro], writes=[res + (0,)], dma_key=ro)
    tc_, rc = tmp(); td, rd = tmp()
    sch.op("dve", (lambda e: e.tensor_tensor(out=tc_[0:32, :], in0=x2, in1=cos, op=ALU.mult)), reads=[r2, "cs"], writes=[rc])
    sch.op("dve", (lambda e: e.tensor_tensor(out=td[0:32, :], in0=x1, in1=sin, op=ALU.mult)), reads=[r1, "cs"], writes=[rd])
    o2 = rot(c, "OB", len(b.OB)); st2 = b.OB[o2]; ro2 = ("OB", o2)
    sch.op("dve", (lambda e: e.tensor_tensor(out=st2[0:32, :], in0=tc_[0:32, :], in1=td[0:32, :], op=ALU.add)), reads=[rc, rd], writes=[ro2])
    sch.op("sp", (lambda e: e.dma_start(out=dst[row0 + 32:row0 + 64, t0:t0 + TT], in_=st2[0:32, :])), reads=[ro2], writes=[res + (1,)], dma_key=ro2)


def inproj_phase(c, l, src_dram, W):
    sch = c.sch
    T = c.T
    groups = win_groups()
    with ExitStack() as es:
        b = Bufs(c, es, wslot_elems=KC * P)
        XN2 = b.sb("XN2", [P, 8, TT], BF16)
        cos_t = b.sb("cos_t", [32, TT], F32)
        sin_t = b.sb("sin_t", [32, TT], F32)
        for tt in range(c.S // TT):
            t0 = tt * TT
            rms_rstd(c, b, src_dram, t0, KC, 7, 0)
            norm_to(c, b, src_dram, t0, KC, lambda kc: c.gains[:, 2 * KC + kc:2 * KC + kc + 1], 0,
                    lambda kc: b.XN[:, kc, :], lambda kc: ("XN", kc))
            sch.op("sp", lambda e, t0=t0: e.dma_start(out=cos_t[:], in_=c.cosT[:, t0:t0 + TT]), writes=["cs"], dma_key="cs0")
            sch.op("sp", lambda e, t0=t0: e.dma_start(out=sin_t[:], in_=c.sinT[:, t0:t0 + TT]), writes=["cs"], dma_key="cs")
            items = [("win", W["win"], gi, KC, P) for gi in range(len(groups))]
            items += [("uq", W["uq"], gi, 8, P) for gi in range(24)]
            items += [("ukv", W["ukv"], gi, 4, P) for gi in range(16)]
            wp = WPipe(c, b, items)
            xn_fn = lambda kc: b.XN[:, kc, :]
            xn_res = lambda kc: ("XN", kc)
            for gi, (nm, i, col0, wd_) in enumerate(groups):
                w, rw = wp.next()
                bank = gi % 4
                res = ("z", nm, i, tt)
                if nm in TOKMAJ:
                    proj_tm(c, b, w, rw, KC, xn_fn, xn_res, bank)
                    vd = {"mv": T["mV"], "dv": T["dV"], "sv": T["sV"]}[nm]
                    store_tm(c, b, bank, vd, t0, i * P, res)
                    continue
                proj_fm(c, b, w, rw, KC, xn_fn, xn_res, bank, wd_)
                if nm == "cq":
                    evac(c, b, bank, P, AF.Copy, T["cqT"][i * P:(i + 1) * P, t0:t0 + TT], False, res)
                elif nm == "ckv":
                    evac(c, b, bank, P, AF.Copy, T["ckvT"][i * P:(i + 1) * P, t0:t0 + TT], False, res)
                elif nm == "kpe":
                    evac(c, b, bank, 32, AF.Copy, T["kpeT"][i * 32:(i + 1) * 32, t0:t0 + TT], False, res)
                elif nm == "mqk":
                    evac(c, b, bank, P, AF.Copy, T["mqkT"][i * P:(i + 1) * P, t0:t0 + TT], False, res)
                elif nm == "mo":
                    evac(c, b, bank, P, AF.Sigmoid, T["moT"][i * P:(i + 1) * P, t0:t0 + TT], False, res)
                elif nm == "mg":
                    evac(c, b, bank, 16, AF.Copy, T["mgT"][0:16, t0:t0 + TT], False, res)
                elif nm == "dq":
                    evac(c, b, bank, P, AF.Copy, T["dqT"][i * P:(i + 1) * P, t0:t0 + TT], True, res)
                elif nm == "dk":
                    evac(c, b, bank, P, AF.Copy, T["dkT"][i * P:(i + 1) * P, t0:t0 + TT], True, res)
                elif nm == "sq":
                    evac(c, b, bank, P, AF.Copy, T["sqT"][i * P:(i + 1) * P, t0:t0 + TT], True, res)
                elif nm == "sk":
                    evac(c, b, bank, P, AF.Copy, T["skT"][0:P, t0:t0 + TT], True, res)
            cqres = lambda kc, tt=tt: ("z", "cq", kc, tt)
            rms_rstd(c, b, T["cqT"], t0, 8, 7, 0, res_fn=cqres)
            norm_to(c, b, T["cqT"], t0, 8, lambda kc: c.vec[:, c.V_QN + kc:c.V_QN + kc + 1], 0,
                    lambda kc: XN2[:, kc, :], lambda kc: ("XN2", kc), res_fn=cqres)
            x2_fn = lambda kc: XN2[:, kc, :]
            x2_res = lambda kc: ("XN2", kc)
            for h in range(8):
                w, rw = wp.next()
                proj_fm(c, b, w, rw, 8, x2_fn, x2_res, 0, P)
                evac(c, b, 0, P, AF.Copy, T["qnT"][h * P:(h + 1) * P, t0:t0 + TT], True, ("qn", h, tt))
                w, rw = wp.next()
                proj_fm(c, b, w, rw, 8, x2_fn, x2_res, 1, 32)
                w, rw = wp.next()
                proj_fm(c, b, w, rw, 8, x2_fn, x2_res, 2, 32)
                rope(c, b, c.ps[1][0:32, :], ("ps", 1), c.ps[2][0:32, :], ("ps", 2), (cos_t[:], sin_t[:]),
                     T["qrT"], h * 64, t0, ("qr", h, tt))
            ckres = lambda kc, tt=tt: ("z", "ckv", kc, tt)
            rms_rstd(c, b, T["ckvT"], t0, 4, 7, 0, res_fn=ckres)
            norm_to(c, b, T["ckvT"], t0, 4, lambda kc: c.vec[:, c.V_KVN + kc:c.V_KVN + kc + 1], 0,
                    lambda kc: XN2[:, kc, :], lambda kc: ("XN2", kc), res_fn=ckres)
            for h in range(8):
                w, rw = wp.next()
                proj_fm(c, b, w, rw, 4, x2_fn, x2_res, 0, P)
                evac(c, b, 0, P, AF.Copy, T["knT"][h * P:(h + 1) * P, t0:t0 + TT], True, ("kn", h, tt))
                w, rw = wp.next()
                proj_tm(c, b, w, rw, 4, x2_fn, x2_res, 3)
                store_tm(c, b, 3, T["aV"], t0, h * P, ("aV", h, tt))
            x1, rx1 = ld_tile(c, b, T["kpeT"][0:32, t0:t0 + TT], rows=32, reads=[("z", "kpe", 0, tt)])
            x2, rx2 = ld_tile(c, b, T["kpeT"][32:64, t0:t0 + TT], rows=32, reads=[("z", "kpe", 1, tt)])
            rope(c, b, x1[0:32, :], rx1, x2[0:32, :], rx2, (cos_t[:], sin_t[:]), T["krT"], 0, t0, ("kr", tt))
    sch.barrier()


def mlstm_prep(c, l):
    sch = c.sch
    T = c.T
    S = c.S
    with ExitStack() as es:
        b = Bufs(c, es, wslot_elems=64, nw=1, xn=False)
        CV = [b.sb("CV%d" % i, [P, TT + 4], F32) for i in range(2)]
        AC = [b.sb("AC%d" % i, [P, TT], F32) for i in range(2)]
        for cf in range(8):
            for tt in range(S // TT):
                t0 = tt * TT
                i = rot(c, "CV", 2); cv = CV[i]; rcv = ("CV", i)
                lo = max(t0 - 2, 0); hi = min(t0 + TT + 2, S)
                if t0 == 0:
                    sch.op("dve", (lambda e, cv=cv: e.memset(cv[:, 0:2], 0.0)), writes=[rcv])
                if t0 + TT == S:
                    sch.op("dve", (lambda e, cv=cv: e.memset(cv[:, TT + 2:TT + 4], 0.0)), writes=[rcv])
                sch.op("sp", (lambda e, cv=cv, lo=lo, hi=hi, t0=t0, cf=cf: e.dma_start(
                    out=cv[:, lo - (t0 - 2):hi - (t0 - 2)], in_=T["mqkT"][cf * P:(cf + 1) * P, lo:hi])),
                    writes=[rcv], dma_key=rcv)
                a0, a1 = AC[0], AC[1]
                wcol = lambda j, cf=cf: c.vec[:, c.V_CW + cf * 5 + j:c.V_CW + cf * 5 + j + 1]
                sch.op("dve", (lambda e, cv=cv, wc=wcol(0): e.tensor_scalar(out=a0[:], in0=cv[:, 0:TT], scalar1=wc, scalar2=None, op0=ALU.mult)),
                       reads=[rcv, "consts"], writes=["AC0"])
                cur, nxt, rc_, rn = a0, a1, "AC0", "AC1"
                for j in range(1, 5):
                    sch.op("dve", (lambda e, cv=cv, j=j, cur=cur, nxt=nxt, wc=wcol(j): e.scalar_tensor_tensor(
                        out=nxt[:], in0=cv[:, j:j + TT], scalar=wc, in1=cur[:], op0=ALU.mult, op1=ALU.add)),
                        reads=[rcv, rc_, "consts"], writes=[rn])
                    cur, nxt, rc_, rn = nxt, cur, rn, rc_
                o = rot(c, "OB", len(b.OB)); st = b.OB[o]; ro = ("OB", o)
                sch.op("act", (lambda e, cur=cur, st=st, cf=cf: e.activation(out=st[:], in_=cur[:], func=AF.Silu,
                                                                            bias=c.vec[:, c.V_CB + cf:c.V_CB + cf + 1])),
                       reads=[rc_, "consts"], writes=[ro])
                sch.op("sp", (lambda e, st=st, cf=cf, t0=t0: e.dma_start(out=T["mqkS"][cf * P:(cf + 1) * P, t0:t0 + TT], in_=st[:])),
                       reads=[ro], dma_key=ro)
        G = [b.sb("G%d" % i, [4, S], F32) for i in range(6)]
        gb = c.gateb

        def ldg(dst, ty):
            sch.op("sp", (lambda e: e.dma_start(out=dst[:], in_=T["mgT"][ty * 4:(ty + 1) * 4, :])), writes=[id(dst)], dma_key=("G", id(dst)))

        def scan(src, tmp, op, reverse):
            sh = 1
            cur, oth = src, tmp
            while sh < S:
                sch.op("act", (lambda e, cur=cur, oth=oth: e.activation(out=oth[:], in_=cur[:], func=AF.Copy)), reads=[id(cur)], writes=[id(oth)])
                if not reverse:
                    sch.op("dve", (lambda e, cur=cur, oth=oth, sh=sh: e.tensor_tensor(out=oth[:, sh:S], in0=cur[:, sh:S], in1=cur[:, 0:S - sh], op=op)),
                           reads=[id(cur)], writes=[id(oth)])
                else:
                    sch.op("dve", (lambda e, cur=cur, oth=oth, sh=sh: e.tensor_tensor(out=oth[:, 0:S - sh], in0=cur[:, 0:S - sh], in1=cur[:, sh:S], op=op)),
                           reads=[id(cur)], writes=[id(oth)])
                cur, oth = oth, cur
                sh *= 2
            return cur, oth

        def outrow(kind, d, src):
            sch.op("sp", (lambda e: e.dma_start(out=T["mrow"][kind, d], in_=src[:])), reads=[id(src)], dma_key=("Gout", kind, d))

        for d in range(2):
            li, lf, t2, t3, t4, t5 = G
            ldg(li, 2 * d); ldg(lf, 2 * d + 1)
            sch.op("dve", (lambda e, d=d: e.tensor_scalar(out=li[:], in0=li[:], scalar1=gb[:, 2 * d:2 * d + 1], scalar2=None, op0=ALU.add)),
                   reads=[id(li), "consts"], writes=[id(li)])
            sch.op("dve", (lambda e, d=d: e.tensor_scalar(out=lf[:], in0=lf[:], scalar1=gb[:, 2 * d + 1:2 * d + 2], scalar2=None, op0=ALU.add)),
                   reads=[id(lf), "consts"], writes=[id(lf)])
            sch.op("act", (lambda e: e.activation(out=lf[:], in_=lf[:], func=AF.Exp, scale=-1.0)), reads=[id(lf)], writes=[id(lf)])
            sch.op("dve", (lambda e: e.tensor_scalar(out=lf[:], in0=lf[:], scalar1=1.0, scalar2=None, op0=ALU.add)), reads=[id(lf)], writes=[id(lf)])
            sch.op("act", (lambda e: e.activation(out=lf[:], in_=lf[:], func=AF.Ln)), reads=[id(lf)], writes=[id(lf)])
            sch.op("dve", (lambda e: e.tensor_scalar(out=lf[:], in0=lf[:], scalar1=-1.0, scalar2=None, op0=ALU.mult)), reads=[id(lf)], writes=[id(lf)])
            sch.op("act", (lambda e: e.activation(out=t2[:], in_=lf[:], func=AF.Copy)), reads=[id(lf)], writes=[id(t2)])
            F, spare = scan(t2, t3, ALU.add, False)
            Bt = t4
            if d == 0:
                A = F
                sch.op("dve", (lambda e, Bt=Bt, li=li, F=F: e.tensor_tensor(out=Bt[:], in0=li[:], in1=F[:], op=ALU.subtract)), reads=[id(li), id(F)], writes=[id(Bt)])
            else:
                sch.op("dve", (lambda e, F=F, lf=lf: e.tensor_tensor(out=F[:], in0=F[:], in1=lf[:], op=ALU.subtract)), reads=[id(F), id(lf)], writes=[id(F)])
                sch.op("dve", (lambda e, Bt=Bt, li=li, F=F: e.tensor_tensor(out=Bt[:], in0=li[:], in1=F[:], op=ALU.add)), reads=[id(li), id(F)], writes=[id(Bt)])
            outrow(0, d, Bt)
            sch.op("act", (lambda e, t5=t5, Bt=Bt: e.activation(out=t5[:], in_=Bt[:], func=AF.Copy)), reads=[id(Bt)], writes=[id(t5)])
            M, sp2 = scan(t5, spare, ALU.max, d == 1)
            if d == 0:
                sch.op("dve", (lambda e, sp2=sp2, F=F, M=M: e.tensor_tensor(out=sp2[:], in0=F[:], in1=M[:], op=ALU.add)), reads=[id(F), id(M)], writes=[id(sp2)])
                sch.op("dve", (lambda e, sp2=sp2: e.tensor_scalar(out=sp2[:], in0=sp2[:], scalar1=-1.0, scalar2=None, op0=ALU.mult)), reads=[id(sp2)], writes=[id(sp2)])
            else:
                sch.op("dve", (lambda e, sp2=sp2, F=F, M=M: e.tensor_tensor(out=sp2[:], in0=F[:], in1=M[:], op=ALU.subtract)), reads=[id(F), id(M)], writes=[id(sp2)])
            outrow(2, d, sp2)
            sch.op("dve", (lambda e, M=M: e.tensor_scalar(out=M[:], in0=M[:], scalar1=-1.0, scalar2=None, op0=ALU.mult)), reads=[id(M)], writes=[id(M)])
            outrow(1, d, M)
    sch.barrier()


class ABufs:
    def __init__(self, c, es):
        nc = c.nc
        c.bufid += 1
        t = "a%d_" % c.bufid
        sb = lambda name, shape, dt: es.enter_context(nc.sbuf_tensor(t + name, shape, dt))
        self.sb = sb
        S = c.S
        self.KT = sb("KT", [P, 2, S], BF16)
        self.V = sb("V", [P, S // P, 256], BF16)
        self.Q = [sb("Q%d" % i, [P, 2, TT], BF16) for i in range(2)]
        self.E = [sb("E%d" % i, [P, TT], F32) for i in range(3)]
        self.ARG = [sb("ARG%d" % i, [P, TT], F32) for i in range(3)]
        self.PT = [sb("PT%d" % i, [P, TT], BF16) for i in range(3)]
        self.TMP = [sb("TMP%d" % i, [P, TT], F32) for i in range(6)]
        self.OS = [sb("OS%d" % i, [P, TT], F32) for i in range(3)]
        self.SQ = [sb("SQ%d" % i, [P, TT], BF16) for i in range(2)]
        self.HACC = sb("HACC", [P, 2, TT], F32)
        self.CONST = sb("CONST", [P, 19, TT], F32)
        self.BCOL = sb("BCOL", [P, S // P], F32)
        self.ROW = [sb("ROW%d" % i, [1, TT], F32) for i in range(4)]
        self.BC = sb("BC", [P, 4], F32)


def attn_tiles(c, A, tiles, qk_fn, nchunk, mode, scale, v_fn, nvh, M, qres, kres, vres):
    sch = c.sch
    n = len(tiles)
    pend = []
    for ti, tl in enumerate(tiles):
        kt = tl["kt"]
        sb_ = ti % 2
        for ch in range(nchunk):
            lhsT, rhs = qk_fn(kt, ch)
            sch.op("pe", (lambda e, lhsT=lhsT, rhs=rhs, ch=ch, sb_=sb_: e.matmul(c.ps[sb_][:], lhsT=lhsT, rhs=rhs, start=(ch == 0), stop=(ch == nchunk - 1))),
                   reads=[qres, kres], writes=[("ps", sb_)])
        pi = rot(c, "PT", len(A.PT)); pt = A.PT[pi]; rp = ("PT", pi)
        if mode == "plain":
            sch.op("act", (lambda e, pt=pt, sb_=sb_: e.activation(out=pt[:], in_=c.ps[sb_][:], func=AF.Exp, scale=scale)),
                   reads=[("ps", sb_)], writes=[rp])
        elif mode == "dist":
            ai = rot(c, "ARG", len(A.ARG)); ar = A.ARG[ai]; ra = ("ARG", ai)
            sch.op("dve", (lambda e, ar=ar, sb_=sb_, tl=tl: e.scalar_tensor_tensor(
                out=ar[:], in0=A.CONST[:, tl["ci"], :], scalar=float(tl["k1"]), in1=c.ps[sb_][:], op0=ALU.mult, op1=ALU.add)),
                reads=[("ps", sb_), "aconst"], writes=[ra])
            if tl["cc"] == 0.0:
                sch.op("act", (lambda e, pt=pt, ar=ar: e.activation(out=pt[:], in_=ar[:], func=AF.Exp, scale=scale)),
                       reads=[ra], writes=[rp])
            else:
                bi = rot(c, "BC", 4); rb_ = ("BC", bi)
                sch.op("dve", (lambda e, bi=bi, tl=tl: e.memset(A.BC[:, bi:bi + 1], float(tl["cc"]))), writes=[rb_])
                sch.op("act", (lambda e, pt=pt, ar=ar, bi=bi: e.activation(out=pt[:], in_=ar[:], func=AF.Exp, scale=scale, bias=A.BC[:, bi:bi + 1])),
                       reads=[ra, rb_], writes=[rp])
        else:
            ei = rot(c, "E", len(A.E)); ee = A.E[ei]; re_ = ("E", ei)
            ai = rot(c, "ARG", len(A.ARG)); ar = A.ARG[ai]; ra = ("ARG", ai)
            if tl.get("mask") is not None:
                sch.op("dve", (lambda e, ar=ar, tl=tl, kt=kt: e.scalar_tensor_tensor(out=ar[:], in0=c.ps[5][:], scalar=A.BCOL[:, kt:kt + 1],
                                                                                in1=A.CONST[:, tl["mask"], :], op0=ALU.add, op1=ALU.add)),
                       reads=[("ps", 5), "aconst", "bcol"], writes=[ra])
            else:
                sch.op("dve", (lambda e, ar=ar, kt=kt: e.tensor_scalar(out=ar[:], in0=c.ps[5][:], scalar1=A.BCOL[:, kt:kt + 1], scalar2=None, op0=ALU.add)),
                       reads=[("ps", 5), "bcol"], writes=[ra])
            sch.op("act", (lambda e, ee=ee, ar=ar: e.activation(out=ee[:], in_=ar[:], func=AF.Exp)), reads=[ra], writes=[re_])
            sch.op("dve", (lambda e, pt=pt, ee=ee, sb_=sb_: e.scalar_tensor_tensor(
                out=pt[:], in0=ee[:], scalar=float(scale), in1=c.ps[sb_][:], op0=ALU.mult, op1=ALU.mult)),
                reads=[re_, ("ps", sb_)], writes=[rp])

        def mk(pt=pt, rp=rp, kt=kt, ti=ti):
            def f():
                for hv in range(nvh):
                    sch.op("pe", (lambda e, hv=hv: e.matmul(c.ps[2 + hv][0:M, :], lhsT=v_fn(kt, hv), rhs=pt[:], start=(ti == 0), stop=(ti == n - 1))),
                           reads=[rp, vres], writes=[("ps", 2 + hv)])
                sch.op("pe", (lambda e: e.matmul(c.ps[4][:], lhsT=c.ones_bf[:], rhs=pt[:], start=(ti == 0), stop=(ti == n - 1))),
                       reads=[rp, "ones_bf"], writes=[("ps", 4)])
            return f
        pend.append(mk())
        if len(pend) > 1:
            pend.pop(0)()
    while pend:
        pend.pop(0)()


def load_k(c, A, slot, src_ap, rows, base, reads=()):
    c.sch.op("sp", (lambda e: e.dma_start(out=A.KT[base:base + rows, slot, :], in_=src_ap)), reads=list(reads), writes=["KT"], dma_key=("KT", slot, base))


def load_v(c, A, vdram, col0, dv):
    src = vdram[:, col0:col0 + dv].rearrange("(kt p) c -> p kt c", p=P)
    c.sch.op("sp", (lambda e: e.dma_start(out=A.V[:, :, 0:dv], in_=src)), writes=["V"], dma_key="V")


def load_q(c, A, slot_list):
    i = rot(c, "Q", 2); q = A.Q[i]; r = ("Q", i)
    for (sl, src, rows, base) in slot_list:
        c.sch.op("sp", (lambda e, sl=sl, src=src, rows=rows, base=base: e.dma_start(out=q[base:base + rows, sl, :], in_=src)),
                 writes=[r], dma_key=("Q", i, sl, base))
    return q, r


def tmp(c, A):
    k = rot(c, "TMP", len(A.TMP)); return A.TMP[k], ("TMP", k)


def store_out(c, A, src_fn, rows, dst_ap):
    o = rot(c, "AOS", len(A.OS)); st = A.OS[o]; r = ("AOS", o)
    src_fn(st, r)
    c.sch.op("sp", (lambda e: e.dma_start(out=dst_ap, in_=st[0:rows, :])), reads=[r], dma_key=r)


def mla_attn(c, A):
    sch = c.sch; T = c.T; S = c.S
    nkt = S // P
    scale = (128 + 64) ** -0.5
    for h in range(8):
        load_k(c, A, 0, T["knT"][h * P:(h + 1) * P, :], P, 0)
        load_k(c, A, 1, T["krT"][0:64, :], 64, 0)
        load_v(c, A, T["aV"], h * P, P)
        for qt in range(S // TT):
            t0 = qt * TT
            q, rq = load_q(c, A, [(0, T["qnT"][h * P:(h + 1) * P, t0:t0 + TT], P, 0), (1, T["qrT"][h * 64:(h + 1) * 64, t0:t0 + TT], 64, 0)])
            def qk(kt, ch, q=q):
                if ch == 0:
                    return A.KT[:, 0, kt * P:(kt + 1) * P], q[:, 0, :]
                return A.KT[0:64, 1, kt * P:(kt + 1) * P], q[0:64, 1, :]
            attn_tiles(c, A, [{"kt": kt} for kt in range(nkt)], qk, 2, "plain", scale,
                       lambda kt, hv: A.V[:, kt, 0:P], 1, P, rq, "KT", "V")
            rz, rrz = tmp(c, A)
            sch.op("dve", (lambda e, rz=rz: e.reciprocal(out=rz[:], in_=c.ps[4][:])), reads=[("ps", 4)], writes=[rrz])
            def fin(st, r, rz=rz, rrz=rrz):
                sch.op("dve", (lambda e: e.tensor_tensor(out=st[:], in0=c.ps[2][:], in1=rz[:], op=ALU.mult)), reads=[("ps", 2), rrz], writes=[r])
            store_out(c, A, fin, P, T["yT"][h * P:(h + 1) * P, t0:t0 + TT])


def diff_attn(c, A):
    sch = c.sch; T = c.T; S = c.S
    nkt = S // P
    scale = 64 ** -0.5
    for h in range(8):
        slope = 2.0 ** (-(h + 1))
        load_k(c, A, 0, T["dkT"][h * P:(h + 1) * P, :], P, 0)
        load_v(c, A, T["dV"], h * P, P)
        for qt in range(S // TT):
            t0 = qt * TT
            q, rq = load_q(c, A, [(0, T["dqT"][h * P:(h + 1) * P, t0:t0 + TT], P, 0)])
            tiles = []
            for kt in range(nkt):
                k0 = kt * P
                r = (k0 - t0) // P
                if 0 <= r <= 3:
                    tiles.append({"kt": kt, "ci": 1 + r, "k1": -slope / scale, "cc": 0.0})
                elif k0 < t0:
                    if slope * (t0 - k0 - 127) > 120.0:
                        continue
                    tiles.append({"kt": kt, "ci": 0, "k1": -slope / scale, "cc": -slope * (t0 - k0)})
                else:
                    if slope * (k0 - t0 - 511) > 120.0:
                        continue
                    tiles.append({"kt": kt, "ci": 0, "k1": slope / scale, "cc": slope * (t0 - k0)})
            d1, rd1 = tmp(c, A)
            for mp in range(2):
                base = 64 * mp
                def qk(kt, ch, q=q, base=base):
                    return A.KT[base:base + 64, 0, kt * P:(kt + 1) * P], q[base:base + 64, 0, :]
                attn_tiles(c, A, tiles, qk, 1, "dist", scale, lambda kt, hv: A.V[:, kt, 0:P], 1, P, rq, "KT", "V")
                rz, rrz = tmp(c, A)
                sch.op("dve", (lambda e, rz=rz: e.reciprocal(out=rz[:], in_=c.ps[4][:])), reads=[("ps", 4)], writes=[rrz])
                if mp == 0:
                    sch.op("dve", (lambda e, rz=rz, d1=d1: e.tensor_tensor(out=d1[:], in0=c.ps[2][:], in1=rz[:], op=ALU.mult)), reads=[("ps", 2), rrz], writes=[rd1])
                else:
                    d2, rd2 = tmp(c, A)
                    sch.op("dve", (lambda e, rz=rz, d2=d2: e.tensor_tensor(out=d2[:], in0=c.ps[2][:], in1=rz[:], op=ALU.mult)), reads=[("ps", 2), rrz], writes=[rd2])
                    sch.op("dve", (lambda e, d2=d2, d1=d1: e.scalar_tensor_tensor(out=d1[:], in0=d2[:], scalar=c.vec[:, c.V_NLAM:c.V_NLAM + 1], in1=d1[:], op0=ALU.mult, op1=ALU.add)),
                           reads=[rd1, rd2, "consts"], writes=[rd1])
            j = rot(c, "ASQ", 2); sq = A.SQ[j]; rs = ("ASQ", j)
            sch.op("act", (lambda e, sq=sq, d1=d1: e.activation(out=sq[:], in_=d1[:], func=AF.Square)), reads=[rd1], writes=[rs])
            sch.op("pe", (lambda e, sq=sq: e.matmul(c.ps[7][:], lhsT=c.ones_bf[:], rhs=sq[:], start=True, stop=True)), reads=[rs, "ones_bf"], writes=[("ps", 7)])
            t1, rt1 = tmp(c, A)
            sch.op("act", (lambda e, t1=t1: e.activation(out=t1[:], in_=c.ps[7][:], func=AF.Sqrt, bias=c.epsc[:, 0:1], scale=1.0 / P)), reads=[("ps", 7)], writes=[rt1])
            sch.op("dve", (lambda e, t1=t1: e.reciprocal(out=t1[:], in_=t1[:])), reads=[rt1], writes=[rt1])
            def fin(st, r, d1=d1, rd1=rd1, t1=t1, rt1=rt1):
                sch.op("dve", (lambda e: e.scalar_tensor_tensor(out=st[:], in0=d1[:], scalar=c.vec[:, c.V_SUBLN:c.V_SUBLN + 1], in1=t1[:], op0=ALU.mult, op1=ALU.mult)),
                       reads=[rd1, rt1, "consts"], writes=[r])
            store_out(c, A, fin, P, T["yT"][2048 + h * P:2048 + (h + 1) * P, t0:t0 + TT])


def swa_attn(c, A):
    sch = c.sch; T = c.T; S = c.S
    nkt = S // P
    scale = 64 ** -0.5
    for g in range(2):
        load_k(c, A, 0, T["skT"][g * 64:(g + 1) * 64, :], 64, 0)
        load_k(c, A, 0, T["skT"][g * 64:(g + 1) * 64, :], 64, 64)
        load_v(c, A, T["sV"], g * 64, 64)
        for r8 in range(8):
            hh = g * 8 + r8
            slope = 2.0 ** (-(hh + 1) / 2.0)
            base = 64 * (hh % 2)
            for qt in range(S // TT):
                t0 = qt * TT
                q, rq = load_q(c, A, [(0, T["sqT"][(hh // 2) * P:(hh // 2 + 1) * P, t0:t0 + TT], P, 0)])
                tiles = []
                for rr in range(6):
                    kt = 4 * qt - 1 + rr
                    if 0 <= kt < nkt:
                        tiles.append({"kt": kt, "ci": 5 + rr, "k1": -slope / scale, "cc": 0.0})
                def qk(kt, ch, q=q, base=base):
                    return A.KT[base:base + 64, 0, kt * P:(kt + 1) * P], q[base:base + 64, 0, :]
                attn_tiles(c, A, tiles, qk, 1, "dist", scale, lambda kt, hv: A.V[:, kt, 0:64], 1, 64, rq, "KT", "V")
                rz, rrz = tmp(c, A)
                sch.op("dve", (lambda e, rz=rz, hh=hh: e.tensor_scalar(out=rz[:], in0=c.ps[4][:], scalar1=c.vec[:, c.V_SINK + hh:c.V_SINK + hh + 1], scalar2=None, op0=ALU.add)),
                       reads=[("ps", 4), "consts"], writes=[rrz])
                sch.op("dve", (lambda e, rz=rz: e.reciprocal(out=rz[:], in_=rz[:])), reads=[rrz], writes=[rrz])
                def fin(st, r, rz=rz, rrz=rrz):
                    sch.op("dve", (lambda e: e.tensor_tensor(out=st[0:64, :], in0=c.ps[2][0:64, :], in1=rz[0:64, :], op=ALU.mult)), reads=[("ps", 2), rrz], writes=[r])
                store_out(c, A, fin, 64, T["yT"][3072 + hh * 64:3072 + (hh + 1) * 64, t0:t0 + TT])


def mlstm_attn(c, A):
    sch = c.sch; T = c.T; S = c.S
    nkt = S // P
    scale = 128 ** -0.5
    for h in range(4):
        load_k(c, A, 0, T["mqkS"][512 + h * P:512 + (h + 1) * P, :], P, 0)
        load_v(c, A, T["mV"], h * 256, 256)
        for d in range(2):
            sch.op("sp", (lambda e, d=d, h=h: e.dma_start(out=A.BCOL[:], in_=T["mrow"][0, d, h].rearrange("(kt p) -> p kt", p=P), allow_slow_non_contiguous=True)),
                   writes=["bcol"], dma_key="bcol")
            for qt in range(S // TT):
                t0 = qt * TT
                q, rq = load_q(c, A, [(0, T["mqkS"][h * P:(h + 1) * P, t0:t0 + TT], P, 0)])
                for kind, bank in ((1, 5), (2, 6)):
                    ri = rot(c, "ROW", len(A.ROW)); row = A.ROW[ri]; rr_ = ("ROW", ri)
                    sch.op("sp", (lambda e, row=row, kind=kind, d=d, h=h, t0=t0: e.dma_start(out=row[:], in_=T["mrow"][kind, d, h:h + 1, t0:t0 + TT])),
                           writes=[rr_], dma_key=rr_)
                    sch.op("pe", (lambda e, row=row, bank=bank: e.matmul(c.ps[bank][:], lhsT=c.ones_row[:], rhs=row[:], start=True, stop=True)),
                           reads=[rr_, "ones_bf"], writes=[("ps", bank)])
                tiles = []
                for kt in range(nkt):
                    r = kt - 4 * qt
                    if 0 <= r <= 3:
                        tiles.append({"kt": kt, "mask": (11 + r) if d == 0 else (15 + r)})
                    elif (r < 0 and d == 0) or (r > 3 and d == 1):
                        tiles.append({"kt": kt, "mask": None})
                def qk(kt, ch, q=q):
                    return A.KT[:, 0, kt * P:(kt + 1) * P], q[:, 0, :]
                attn_tiles(c, A, tiles, qk, 1, "mlstm", scale, lambda kt, hv: A.V[:, kt, hv * P:(hv + 1) * P], 2, P, rq, "KT", "V")
                da, rda = tmp(c, A)
                sch.op("dve", (lambda e, da=da: e.tensor_scalar(out=da[:], in0=c.ps[4][:], scalar1=-1.0, scalar2=None, op0=ALU.mult)), reads=[("ps", 4)], writes=[rda])
                sch.op("dve", (lambda e, da=da: e.tensor_tensor(out=da[:], in0=da[:], in1=c.ps[4][:], op=ALU.max)), reads=[("ps", 4), rda], writes=[rda])
                en, ren = tmp(c, A)
                sch.op("act", (lambda e, en=en: e.activation(out=en[:], in_=c.ps[6][:], func=AF.Exp)), reads=[("ps", 6)], writes=[ren])
                sch.op("dve", (lambda e, da=da, en=en: e.tensor_tensor(out=da[:], in0=da[:], in1=en[:], op=ALU.max)), reads=[rda, ren], writes=[rda])
                sch.op("dve", (lambda e, da=da: e.reciprocal(out=da[:], in_=da[:])), reads=[rda], writes=[rda])
                for hv in range(2):
                    if d == 0:
                        sch.op("dve", (lambda e, hv=hv, da=da: e.tensor_tensor(out=A.HACC[:, hv, :], in0=c.ps[2 + hv][:], in1=da[:], op=ALU.mult)),
                               reads=[("ps", 2 + hv), rda], writes=[("HACC", hv, qt)])
                        hst, rhst = tmp(c, A)
                        sch.op("act", (lambda e, hv=hv, hst=hst: e.activation(out=hst[:], in_=A.HACC[:, hv, :], func=AF.Copy)), reads=[("HACC", hv, qt)], writes=[rhst])
                        sch.op("sp", (lambda e, hv=hv, hst=hst, h=h, t0=t0: e.dma_start(out=T["mhf"][h * 256 + hv * P:h * 256 + (hv + 1) * P, t0:t0 + TT], in_=hst[:])),
                               reads=[rhst], writes=[("mhf", h, hv, qt)], dma_key=rhst)
                    else:
                        hb, rhb = tmp(c, A)
                        sch.op("dve", (lambda e, hv=hv, da=da, hb=hb: e.tensor_tensor(out=hb[:], in0=c.ps[2 + hv][:], in1=da[:], op=ALU.mult)),
                               reads=[("ps", 2 + hv), rda], writes=[rhb])
                        hf, rhf = tmp(c, A)
                        sch.op("sp", (lambda e, hv=hv, hf=hf, h=h, t0=t0: e.dma_start(out=hf[:], in_=T["mhf"][h * 256 + hv * P:h * 256 + (hv + 1) * P, t0:t0 + TT])),
                               reads=[("mhf", h, hv, qt)], writes=[rhf], dma_key=rhf)
                        og, rog = tmp(c, A)
                        sch.op("sp", (lambda e, hv=hv, og=og, h=h, t0=t0: e.dma_start(out=og[:], in_=T["moT"][h * 256 + hv * P:h * 256 + (hv + 1) * P, t0:t0 + TT])),
                               writes=[rog], dma_key=rog)
                        sch.op("dve", (lambda e, hb=hb, hf=hf: e.tensor_tensor(out=hb[:], in0=hb[:], in1=hf[:], op=ALU.add)), reads=[rhb, rhf], writes=[rhb])
                        def fin(st, r, hb=hb, rhb=rhb, og=og, rog=rog):
                            sch.op("dve", (lambda e: e.tensor_tensor(out=st[:], in0=hb[:], in1=og[:], op=ALU.mult)), reads=[rhb, rog], writes=[r])
                        store_out(c, A, fin, P, T["yT"][1024 + h * 256 + hv * P:1024 + h * 256 + (hv + 1) * P, t0:t0 + TT])


def attn_phase(c, l):
    sch = c.sch
    with ExitStack() as es:
        A = ABufs(c, es)
        sch.op("sp", lambda e: e.dma_start(out=A.CONST[:], in_=c.aconst), writes=["aconst"], dma_key="aconst")
        mla_attn(c, A)
        mlstm_attn(c, A)
        diff_attn(c, A)
        swa_attn(c, A)
    sch.barrier()


def out_phase(c, l, src_dram, dst_dram, wout):
    sch = c.sch; T = c.T
    with ExitStack() as es:
        b = Bufs(c, es, wslot_elems=KC * P)
        for tt in range(c.S // TT):
            t0 = tt * TT
            for gi in range(4):
                rms_rstd(c, b, T["yT"][gi * 1024:(gi + 1) * 1024, :], t0, 8, 7, 0)
                norm_to(c, b, T["yT"][gi * 1024:(gi + 1) * 1024, :], t0, 8,
                        lambda kc, gi=gi: c.vec[:, c.V_GN + gi * 8 + kc:c.V_GN + gi * 8 + kc + 1], 0,
                        lambda kc, gi=gi: b.XN[:, gi * 8 + kc, :], lambda kc, gi=gi: ("XN", gi * 8 + kc))
            wp = WPipe(c, b, [("wout", wout, m, KC, P) for m in range(KC)])
            post_proj(c, b, wp, lambda kc: b.XN[:, kc, :], lambda kc: ("XN", kc), KC,
                      lambda m: c.gains[:, 3 * KC + m:3 * KC + m + 1], 1.0, src_dram, dst_dram, c.hT, t0)
    sch.barrier()


NVIN = 367
NV = 385
WSPEC = [("win", 73, KC * P), ("uq", 24, 8 * P), ("ukv", 16, 4 * P), ("wout", KC, KC * P),
         ("f1gu", 2 * NDFF, KC * P), ("f1dn", KC, NDFF * P), ("f2gu", 2 * NDFF, KC * P), ("f2dn", KC, NDFF * P)]


def build(S, L):
    nc = bass.Bass("TRN2", target_bir_lowering=False)
    c = Ctx()
    c.nc = nc; c.S = S; c.sch = Sched(nc); c.rot = {}; c.bufid = 0
    sch = c.sch
    din = lambda name, shape, dt=F32: nc.dram_tensor(name, shape, dt, kind="ExternalInput").ap()
    dsc = lambda name, shape, dt: nc.dram_tensor(name, shape, dt).ap()
    xT = din("xT", [D, S])
    yout = nc.dram_tensor("yT_out", [D, S], F32, kind="ExternalOutput").ap()
    gains_in = din("gains", [L, P, 6 * KC])
    vec_in = din("vec", [L, P, NVIN])
    gateb_in = din("gateb", [L, 4, 4])
    c.cosT = din("cosT", [32, S]); c.sinT = din("sinT", [32, S])
    c.aconst = din("aconst", [P, 19, TT])
    Win = {n: din(n, [L, ng, P, row]) for (n, ng, row) in WSPEC}
    npar = 2 if L > 1 else 1
    Wbf2 = [{n: dsc(n + "_bf%d" % par, [ng, P, row], BF16) for (n, ng, row) in WSPEC} for par in range(npar)]

    def cast_layer(l):
        par = l % npar
        for (n, ng, row) in WSPEC:
            cast_weight(c, n + str(par), Win[n][l], Wbf2[par][n], ng, row)
    T = {}
    for n, rows, dt in [("cqT", 1024, F32), ("ckvT", 512, F32), ("kpeT", 64, F32), ("mqkT", 1024, F32), ("moT", 1024, F32),
                        ("mgT", 16, F32), ("dqT", 1024, BF16), ("dkT", 1024, BF16), ("sqT", 1024, BF16), ("skT", 128, BF16),
                        ("qnT", 1024, BF16), ("qrT", 512, BF16), ("knT", 1024, BF16), ("krT", 64, BF16), ("mqkS", 1024, BF16),
                        ("yT", 4096, F32), ("mhf", 1024, F32)]:
        if Ctx.debug and n in ("yT", "mqkS", "moT", "mhf", "mqkT", "mgT"):
            T[n] = nc.dram_tensor("t_" + n, [rows, S], dt, kind="ExternalOutput").ap()
        else:
            T[n] = dsc("t_" + n, [rows, S], dt)
    for n, cols in [("mV", 1024), ("dV", 1024), ("sV", 128), ("aV", 1024)]:
        if Ctx.debug and n == "mV":
            T[n] = nc.dram_tensor("t_" + n, [S, cols], BF16, kind="ExternalOutput").ap()
        else:
            T[n] = dsc("t_" + n, [S, cols], BF16)
    if Ctx.debug:
        T["mrow"] = nc.dram_tensor("t_mrow", [3, 2, 4, S], F32, kind="ExternalOutput").ap()
    else:
        T["mrow"] = dsc("t_mrow", [3, 2, 4, S], F32)
    c.T = T
    c.hT = dsc("hT", [D, TT], F32)
    xA = dsc("xA", [D, S], F32); xB = dsc("xB", [D, S], F32); xC = dsc("xC", [D, S], F32)
    c.V_QN, c.V_KVN, c.V_CW, c.V_CB, c.V_GN = 0, 8, 12, 52, 60
    V_SUBRAW, V_LAMINIT, V_OML, V_SINKRAW, V_LAMVEC = 92, 93, 94, 95, 111
    c.V_SUBLN, c.V_NLAM, c.V_SINK = 367, 368, 369
    with ExitStack() as es:
        c.ps = [es.enter_context(nc.psum_tensor("ps%d" % i, [P, 512], F32)) for i in range(8)]
        sbg = lambda name, shape, dt: es.enter_context(nc.sbuf_tensor(name, shape, dt))
        c.ones_bf = sbg("ones_bf", [P, P], BF16)
        c.ones_row = sbg("ones_row", [1, P], F32)
        c.gains = sbg("gains_sb", [P, 6 * KC], F32)
        c.vec = sbg("vec_sb", [P, NV], F32)
        c.gateb = sbg("gateb_sb", [4, 4], F32)
        vt = sbg("vtmp", [P, 64], F32)
        c.epsc = sbg("epsc", [P, 1], F32)
        sch.op("dve", lambda e: e.memset(c.epsc[:], EPS), writes=["ones_bf"])
        vs = sbg("vsum", [P, 4], F32)
        sch.op("dve", lambda e: e.memset(c.ones_bf[:], 1.0), writes=["ones_bf"])
        sch.op("dve", lambda e: e.memset(c.ones_row[:], 1.0), writes=["ones_bf"])
        cur = xT
        for l in range(L):
            if l == 0:
                cast_layer(0)
            if l + 1 < L:
                cast_layer(l + 1)
            Wbf = Wbf2[l % npar]
            c.wsuf = str(l % npar)
            sch.op("sp", lambda e, l=l: e.dma_start(out=c.gains[:], in_=gains_in[l]), writes=["consts"], dma_key="cg")
            sch.op("sp", lambda e, l=l: e.dma_start(out=c.vec[:, 0:NVIN], in_=vec_in[l]), writes=["consts"], dma_key="cv")
            sch.op("sp", lambda e, l=l: e.dma_start(out=c.gateb[:], in_=gateb_in[l]), writes=["consts"], dma_key="cgb")
            v = c.vec
            sch.op("dve", lambda e: e.tensor_tensor(out=v[:, c.V_SUBLN:c.V_SUBLN + 1], in0=v[:, V_SUBRAW:V_SUBRAW + 1], in1=v[:, V_OML:V_OML + 1], op=ALU.mult),
                   reads=["consts"], writes=["consts"])
            for k in range(2):
                sch.op("dve", lambda e, k=k: e.tensor_tensor(out=vt[:], in0=v[:, V_LAMVEC + 128 * k:V_LAMVEC + 128 * k + 64],
                                                              in1=v[:, V_LAMVEC + 128 * k + 64:V_LAMVEC + 128 * k + 128], op=ALU.mult),
                       reads=["consts"], writes=["vt"])
                sch.op("dve", lambda e, k=k: e.reduce_sum(out=vs[:, k:k + 1], in_=vt[:], axis=mybir.AxisListType.X), reads=["vt"], writes=["vs"])
            sch.op("act", lambda e: e.activation(out=vs[:, 0:2], in_=vs[:, 0:2], func=AF.Exp), reads=["vs"], writes=["vs"])
            sch.op("dve", lambda e: e.tensor_tensor(out=vs[:, 2:3], in0=vs[:, 1:2], in1=vs[:, 0:1], op=ALU.subtract), reads=["vs"], writes=["vs"])
            sch.op("dve", lambda e: e.tensor_tensor(out=v[:, c.V_NLAM:c.V_NLAM + 1], in0=vs[:, 2:3], in1=v[:, V_LAMINIT:V_LAMINIT + 1], op=ALU.subtract),
                   reads=["vs", "consts"], writes=["consts"])
            sch.op("act", lambda e: e.activation(out=v[:, c.V_SINK:c.V_SINK + 16], in_=v[:, V_SINKRAW:V_SINKRAW + 16], func=AF.Exp),
                   reads=["consts"], writes=["consts"])
            last = (l == L - 1)
            ffn_phase(c, cur, (xA if c.stages >= 2 else yout), Wbf["f1gu"], Wbf["f1dn"], c.gains, 0, 1, "f1")
            if c.stages >= 2:
                inproj_phase(c, l, xA, Wbf)
                mlstm_prep(c, l)
                attn_phase(c, l)
                out_phase(c, l, xA, xB, Wbf["wout"])
                ffn_phase(c, xB, (yout if last else xC), Wbf["f2gu"], Wbf["f2dn"], c.gains, 4, 5, "f2")
            cur = xC
        c.n_dma_keys = len(sch.dma_keys)
        sch.emit()
    return nc, c


Ctx.stages = 2
Ctx.debug = False


def relayout(W, cols_list):
    K = W.shape[0]
    out = np.zeros((len(cols_list), P, K // P, P), np.float32)
    for g, (c0, w) in enumerate(cols_list):
        out[g, :, :, :w] = W[:, c0:c0 + w].reshape(K // P, P, w).transpose(1, 0, 2)
    return out.reshape(len(cols_list), P, -1)


def relayout_uniform(W):
    K, N = W.shape
    return np.ascontiguousarray(W.reshape(K // P, P, N // P, P).transpose(2, 1, 0, 3)).reshape(N // P, P, -1)


def layer_inputs(inp, l):
    o = {}
    o["win"] = relayout(inp["w_in"][l], [(c0, w) for (_, _, c0, w) in win_groups()])
    uqc = []
    for h in range(8):
        uqc += [(h * 192, 128), (h * 192 + 128, 32), (h * 192 + 160, 32)]
    o["uq"] = relayout(inp["mla_w_uq"][l], uqc)
    kvc = []
    for h in range(8):
        kvc += [(h * 256, 128), (h * 256 + 128, 128)]
    o["ukv"] = relayout(inp["mla_w_ukv"][l], kvc)
    o["wout"] = relayout_uniform(inp["w_out"][l])
    o["f1gu"] = relayout_uniform(inp["ffn1_w_gu"][l]); o["f1dn"] = relayout_uniform(inp["ffn1_w_down"][l])
    o["f2gu"] = relayout_uniform(inp["ffn2_w_gu"][l]); o["f2dn"] = relayout_uniform(inp["ffn2_w_down"][l])
    o["gains"] = np.ascontiguousarray(inp["norm_gains"][l].reshape(6, KC, P).transpose(2, 0, 1)).reshape(P, 6 * KC)
    vec = np.zeros((P, NVIN), np.float32)
    vec[:, 0:8] = inp["mla_q_norm"][l].reshape(8, P).T
    vec[:, 8:12] = inp["mla_kv_norm"][l].reshape(4, P).T
    vec[:, 12:52] = inp["mlstm_conv_w"][l].reshape(5, 8, P).transpose(2, 1, 0).reshape(P, 40)
    vec[:, 52:60] = inp["mlstm_conv_b"][l].reshape(8, P).T
    vec[:, 60:92] = inp["group_norm"][l].reshape(32, P).T
    vec[:, 92] = inp["diff_subln"][l]
    lam_init = 0.8 - 0.6 * math.exp(-0.3 * l)
    vec[:, 93] = lam_init
    vec[:, 94] = 1.0 - lam_init
    vec[:, 95:111] = inp["swa_sink"][l][None, :]
    vec[:, 111:367] = inp["diff_lambda"][l].reshape(1, 256)
    o["vec"] = vec
    o["gateb"] = np.ascontiguousarray(inp["mlstm_gate_b"][l].T)
    return o


def const_inputs(S):
    inv = 10000.0 ** (-np.arange(32, dtype=np.float32) / 32)
    ang = np.arange(S, dtype=np.float32)[None, :] * inv[:, None]
    k = np.arange(P, dtype=np.float32)[:, None]
    q = np.arange(TT, dtype=np.float32)[None, :]
    ac = np.zeros((P, 19, TT), np.float32)
    ac[:, 0] = q - k
    for r in range(4):
        ac[:, 1 + r] = np.abs(q - k - 128 * r)
        ac[:, 11 + r] = np.where(128 * r + k <= q, 0.0, NEG)
        ac[:, 15 + r] = np.where(128 * r + k >= q, 0.0, NEG)
    for rr in range(6):
        dist = np.abs(q - (rr - 1) * 128 - k)
        ac[:, 5 + rr] = np.where(dist <= 128, dist, 1.0e6)
    return {"cosT": np.cos(ang).astype(np.float32), "sinT": np.sin(ang).astype(np.float32), "aconst": ac}


_PROG = {}


def kernel(**inp):
    inp = {k: np.asarray(v) for k, v in inp.items()}
    x = inp["x"]
    B, S, _ = x.shape
    L = inp["w_in"].shape[0]
    key = (S, L)
    if key not in _PROG:
        _PROG[key] = build(S, L)[0]
    nc = _PROG[key]
    shared = const_inputs(S)
    per_layer = [layer_inputs(inp, l) for l in range(L)]
    for k in per_layer[0]:
        shared[k] = np.stack([pl[k] for pl in per_layer], axis=0)
    maps = []
    for b in range(B):
        m = dict(shared)
        m["xT"] = np.ascontiguousarray(x[b].T)
        maps.append(m)
    res = run_bass_kernel_spmd(nc, maps, core_ids=list(range(B)))
    return np.stack([np.asarray(res.results[b]["yT_out"]).T for b in range(B)], axis=0).astype(np.float32)
```

```python
import math
import numpy as np
from contextlib import ExitStack
import concourse.bass as bass
import concourse.mybir as mybir
from concourse.bass_utils import run_bass_kernel_spmd

F32 = mybir.dt.float32
BF16 = mybir.dt.bfloat16
AF = mybir.ActivationFunctionType
ALU = mybir.AluOpType

D = 4096
P = 128
KC = D // P
TT = 512
EPS = 1e-6
DFF = 6144
NDFF = DFF // P
IN_COLS = 9040
OFF_A, OFF_B, OFF_C, OFF_D = 0, 1600, 4688, 7760
NEG = -1.0e30
AHEAD = 3

COMPUTE = ("pe", "act", "dve", "pool")
ENGS = ("pe", "act", "dve", "pool", "sp")


class Rec:
    __slots__ = ("eng", "fn", "deps", "dma", "key", "kidx", "sig", "cnt")

    def __init__(self, eng, fn, deps, dma, key, kidx):
        self.eng = eng; self.fn = fn; self.deps = deps; self.dma = dma
        self.key = key; self.kidx = kidx; self.sig = False; self.cnt = 0


class Sched:
    def __init__(self, nc):
        self.nc = nc
        self.streams = {e: [] for e in ENGS}
        self.last_w = {}
        self.readers = {}
        self.dma_keys = {}
        self.last_by_key = {}
        self.pending = {}

    def op(self, eng, fn, reads=(), writes=(), dma_key=None):
        deps = []
        lw = self.last_w; rd = self.readers
        for r in reads:
            w = lw.get(r)
            if w is not None:
                deps.append(w)
        for r in writes:
            w = lw.get(r)
            if w is not None:
                deps.append(w)
            rs = rd.get(r)
            if rs:
                deps.extend(rs)
        pb = self.pending.pop(eng, None)
        if pb:
            deps.extend(pb)
        dma = dma_key is not None
        kidx = 0
        if dma:
            kidx = self.dma_keys.get(dma_key, 0)
            self.dma_keys[dma_key] = kidx + 1
        rec = Rec(eng, fn, deps, dma, dma_key, kidx)
        if dma:
            self.last_by_key[dma_key] = rec
        for d in deps:
            d.sig = True
        for r in writes:
            lw[r] = rec
            rd[r] = []
        for r in reads:
            l = rd.get(r)
            if l is None:
                rd[r] = [rec]
            else:
                l.append(rec)
        self.streams[eng].append(rec)
        return rec

    def barrier(self):
        recs = []
        for e in ENGS:
            for r in reversed(self.streams[e]):
                if not r.dma:
                    recs.append(r)
                    break
        recs += list(self.last_by_key.values())
        self.pending = {e: list(recs) for e in ENGS}
        self.last_w = {}
        self.readers = {}

    def emit(self):
        nc = self.nc
        with ExitStack() as es:
            esem = {e: es.enter_context(nc.semaphore("sem_" + e)) for e in COMPUTE}
            ksem = {}
            for i, k in enumerate(self.dma_keys):
                ksem[k] = es.enter_context(nc.semaphore("semk_%d" % i))
            for e in COMPUTE:
                c = 0
                for r in self.streams[e]:
                    if r.sig and not r.dma:
                        c += 1
                        r.cnt = c
            streams = self.streams

            def run(ename, eng):
                seen = {}
                for r in streams[ename]:
                    need = {}
                    for d in r.deps:
                        if d.dma:
                            s = ("k", d.key); v = 16 * (d.kidx + 1)
                        else:
                            if d.eng == ename and ename == "pe":
                                continue
                            s = ("e", d.eng); v = d.cnt
                        if seen.get(s, 0) >= v:
                            continue
                        if need.get(s, 0) < v:
                            need[s] = v
                    for s, v in need.items():
                        seen[s] = v
                        eng.wait_ge(ksem[s[1]] if s[0] == "k" else esem[s[1]], v)
                    ins = r.fn(eng)
                    if r.dma:
                        ins.then_inc(ksem[r.key], 16)
                    elif r.sig:
                        ins.then_inc(esem[ename], 1)
                if ename == "sp":
                    for k, n in self.dma_keys.items():
                        eng.wait_ge(ksem[k], 16 * n)

            with nc.Block() as block:
                @block.tensor
                def _(e): run("pe", e)

                @block.scalar
                def _(e): run("act", e)

                @block.vector
                def _(e): run("dve", e)

                @block.gpsimd
                def _(e): run("pool", e)

                @block.sync
                def _(e): run("sp", e)


class Ctx:
    pass


def rot(c, name, n):
    i = c.rot.get(name, 0)
    c.rot[name] = i + 1
    return i % n


class Bufs:
    def __init__(self, c, es, wslot_elems=4096, nw=4, xn=True, actt=False):
        nc = c.nc
        c.bufid += 1
        t = "b%d_" % c.bufid
        sb = lambda name, shape, dt: es.enter_context(nc.sbuf_tensor(t + name, shape, dt))
        self.sb = sb
        self.wslots = [sb("w%d" % i, [P, wslot_elems], BF16) for i in range(nw)]
        if xn:
            self.XN = sb("XN", [P, KC, TT], BF16)
        if actt:
            self.ACTT = sb("ACTT", [P, NDFF, TT], BF16)
        self.XS = [sb("XS%d" % i, [P, TT], F32) for i in range(4)]
        self.HS = [sb("HS%d" % i, [P, TT], F32) for i in range(3)]
        self.OS = [sb("OS%d" % i, [P, TT], F32) for i in range(3)]
        self.OB = [sb("OB%d" % i, [P, TT], BF16) for i in range(3)]
        self.SQ = [sb("SQ%d" % i, [P, TT], BF16) for i in range(3)]
        self.SG = [sb("SG%d" % i, [P, TT], F32) for i in range(3)]
        self.T1 = sb("T1", [P, TT], F32)
        self.R = [sb("R%d" % i, [P, TT], F32) for i in range(2)]


def cast_weight(c, name, w_in, w_bf, ng, row_elems):
    sch = c.sch
    bb = 2048
    while row_elems % bb:
        bb //= 2
    recs = []
    for g in range(ng):
        src = w_in[g].rearrange("p (a b) -> p a b", b=bb)
        dst = w_bf[g].rearrange("p (a b) -> p a b", b=bb)
        recs.append(sch.op("pool", (lambda e, s=src, d=dst: e.dma_start(out=d, in_=s)),
                           writes=[("wbf", name, g)], dma_key=("cast", name)))
    for r in recs:
        r.kidx = recs[-1].kidx


def load_w(c, b, name, w_bf, g, nkc, gw):
    i = rot(c, "w", len(b.wslots))
    slot = b.wslots[i]
    n = nkc * gw
    res = ("wslot", i)
    c.sch.op("sp", (lambda e, s=slot, src=w_bf[g], n=n: e.dma_start(out=s[:, 0:n], in_=src)),
             reads=[("wbf", name + c.wsuf, g)], writes=[res], dma_key=("wslot", i))
    return slot[:, 0:n].rearrange("p (k c) -> p k c", c=gw), res


class WPipe:
    def __init__(self, c, b, items):
        self.c = c; self.b = b; self.items = items; self.issued = []; self.pos = 0

    def next(self):
        ahead = len(self.b.wslots) - 1
        while len(self.issued) < min(len(self.items), self.pos + 1 + ahead):
            name, w_bf, g, nkc, gw = self.items[len(self.issued)]
            self.issued.append(load_w(self.c, self.b, name, w_bf, g, nkc, gw))
        r = self.issued[self.pos]
        self.pos += 1
        return r


def ld_tile(c, b, src_ap, rows=P, cols=TT, reads=()):
    i = rot(c, "XS", len(b.XS)); xs = b.XS[i]
    c.sch.op("sp", (lambda e: e.dma_start(out=xs[0:rows, 0:cols], in_=src_ap)),
             reads=list(reads), writes=[("XS", i)], dma_key=("XS", i))
    return xs, ("XS", i)


def rms_rstd(c, b, src_dram, t0, nchunks, ps_bank, ridx, res_fn=None):
    sch = c.sch
    ps = c.ps[ps_bank]
    for kc in range(nchunks):
        xs, rx = ld_tile(c, b, src_dram[kc * P:(kc + 1) * P, t0:t0 + TT], reads=([res_fn(kc)] if res_fn else ()))
        j = rot(c, "SQ", len(b.SQ)); sq = b.SQ[j]
        sch.op("act", (lambda e, xs=xs, sq=sq: e.activation(out=sq[:], in_=xs[:], func=AF.Square)),
               reads=[rx], writes=[("SQ", j)])
        sch.op("pe", (lambda e, sq=sq, kc=kc: e.matmul(ps[:], lhsT=c.ones_bf[:], rhs=sq[:], start=(kc == 0), stop=(kc == nchunks - 1))),
               reads=[("SQ", j), "ones_bf"], writes=[("ps", ps_bank)])
    nfeat = nchunks * P
    sch.op("act", (lambda e: e.activation(out=b.T1[:], in_=ps[:], func=AF.Sqrt, bias=c.epsc[:, 0:1], scale=1.0 / nfeat)),
           reads=[("ps", ps_bank)], writes=["T1"])
    sch.op("dve", (lambda e: e.reciprocal(out=b.R[ridx][:], in_=b.T1[:])), reads=["T1"], writes=[("R", ridx)])


def norm_to(c, b, src_dram, t0, nchunks, gain_ap_fn, ridx, dst_fn, dst_res_fn, res_fn=None):
    for kc in range(nchunks):
        xs, rx = ld_tile(c, b, src_dram[kc * P:(kc + 1) * P, t0:t0 + TT], reads=([res_fn(kc)] if res_fn else ()))
        c.sch.op("dve", (lambda e, xs=xs, kc=kc: e.scalar_tensor_tensor(
            out=dst_fn(kc), in0=xs[:], scalar=gain_ap_fn(kc), in1=b.R[ridx][:], op0=ALU.mult, op1=ALU.mult)),
            reads=[rx, ("R", ridx), "consts"], writes=[dst_res_fn(kc)])


def post_proj(c, b, wp, rhs_fn, rhs_res_fn, nk, gain_fn, factor, src_dram, dst_dram, hT, t0):
    sch = c.sch
    pend = None
    for m in range(KC):
        w, rw = wp.next()
        bk = 4 + (m % 2)
        for kc in range(nk):
            sch.op("pe", (lambda e, w=w, kc=kc, bk=bk: e.matmul(c.ps[bk][:], lhsT=w[:, kc, :], rhs=rhs_fn(kc), start=(kc == 0), stop=(kc == nk - 1))),
                   reads=[rw, rhs_res_fn(kc)], writes=[("ps", bk)])
        if pend is not None:
            pend()
        i = rot(c, "HS", len(b.HS)); hs = b.HS[i]
        sch.op("act", (lambda e, hs=hs, bk=bk: e.activation(out=hs[:], in_=c.ps[bk][:], func=AF.Copy)),
               reads=[("ps", bk)], writes=[("HS", i)])
        j = rot(c, "SQ", len(b.SQ)); sq = b.SQ[j]
        sch.op("act", (lambda e, sq=sq, bk=bk: e.activation(out=sq[:], in_=c.ps[bk][:], func=AF.Square)),
               reads=[("ps", bk)], writes=[("SQ", j)])
        sch.op("sp", (lambda e, hs=hs, m=m: e.dma_start(out=hT[m * P:(m + 1) * P, :], in_=hs[:])),
               reads=[("HS", i)], writes=[("hT", m)], dma_key=("HS", i))

        def mk(sq=sq, j=j, m=m):
            def f():
                sch.op("pe", (lambda e: e.matmul(c.ps[6][:], lhsT=c.ones_bf[:], rhs=sq[:], start=(m == 0), stop=(m == KC - 1))),
                       reads=[("SQ", j), "ones_bf"], writes=[("ps", 6)])
            return f
        pend = mk()
    pend()
    sch.op("act", (lambda e: e.activation(out=b.T1[:], in_=c.ps[6][:], func=AF.Sqrt, bias=c.epsc[:, 0:1], scale=1.0 / D)),
           reads=[("ps", 6)], writes=["T1"])
    sch.op("dve", (lambda e: e.reciprocal(out=b.R[1][:], in_=b.T1[:])), reads=["T1"], writes=[("R", 1)])
    for m in range(KC):
        i = rot(c, "HS", len(b.HS)); hs = b.HS[i]
        sch.op("sp", (lambda e, hs=hs, m=m: e.dma_start(out=hs[:], in_=hT[m * P:(m + 1) * P, :])),
               reads=[("hT", m)], writes=[("HS", i)], dma_key=("HS", i))
        xs, rx = ld_tile(c, b, src_dram[m * P:(m + 1) * P, t0:t0 + TT])
        k = rot(c, "SG", len(b.SG)); sg = b.SG[k]
        sch.op("dve", (lambda e, hs=hs, sg=sg, m=m: e.scalar_tensor_tensor(
            out=sg[:], in0=hs[:], scalar=gain_fn(m), in1=b.R[1][:], op0=ALU.mult, op1=ALU.mult)),
            reads=[("HS", i), ("R", 1), "consts"], writes=[("SG", k)])
        o = rot(c, "OS", len(b.OS)); os_ = b.OS[o]
        sch.op("dve", (lambda e, sg=sg, xs=xs, os_=os_: e.scalar_tensor_tensor(
            out=os_[:], in0=sg[:], scalar=float(factor), in1=xs[:], op0=ALU.mult, op1=ALU.add)),
            reads=[("SG", k), rx], writes=[("OS", o)])
        sch.op("sp", (lambda e, os_=os_, m=m: e.dma_start(out=dst_dram[m * P:(m + 1) * P, t0:t0 + TT], in_=os_[:])),
               reads=[("OS", o)], writes=[("act", id(dst_dram), m, t0)], dma_key=("OS", o))


def ffn_phase(c, src_dram, dst_dram, wgu, wd, gains, g_pre, g_post, wname):
    sch = c.sch
    with ExitStack() as es:
        b = Bufs(c, es, wslot_elems=NDFF * P, actt=True)
        for tt in range(c.S // TT):
            t0 = tt * TT
            rms_rstd(c, b, src_dram, t0, KC, 7, 0)
            norm_to(c, b, src_dram, t0, KC, lambda kc: gains[:, g_pre * KC + kc:g_pre * KC + kc + 1], 0,
                    lambda kc: b.XN[:, kc, :], lambda kc: ("XN", kc))
            items = []
            for j in range(NDFF):
                items.append((wname + "gu", wgu, j, KC, P)); items.append((wname + "gu", wgu, NDFF + j, KC, P))
            for m in range(KC):
                items.append((wname + "dn", wd, m, NDFF, P))
            wp = WPipe(c, b, items)
            for j in range(NDFF):
                bg = 2 * (j % 2); bu = bg + 1
                wg, rg = wp.next()
                for kc in range(KC):
                    sch.op("pe", (lambda e, wg=wg, kc=kc, bg=bg: e.matmul(c.ps[bg][:], lhsT=wg[:, kc, :], rhs=b.XN[:, kc, :], start=(kc == 0), stop=(kc == KC - 1))),
                           reads=[rg, ("XN", kc)], writes=[("ps", bg)])
                wu, ru = wp.next()
                for kc in range(KC):
                    sch.op("pe", (lambda e, wu=wu, kc=kc, bu=bu: e.matmul(c.ps[bu][:], lhsT=wu[:, kc, :], rhs=b.XN[:, kc, :], start=(kc == 0), stop=(kc == KC - 1))),
                           reads=[ru, ("XN", kc)], writes=[("ps", bu)])
                k = rot(c, "SG", len(b.SG)); sg = b.SG[k]
                sch.op("act", (lambda e, sg=sg, bg=bg: e.activation(out=sg[:], in_=c.ps[bg][:], func=AF.Silu)),
                       reads=[("ps", bg)], writes=[("SG", k)])
                sch.op("dve", (lambda e, sg=sg, bu=bu, j=j: e.tensor_tensor(out=b.ACTT[:, j, :], in0=sg[:], in1=c.ps[bu][:], op=ALU.mult)),
                       reads=[("SG", k), ("ps", bu)], writes=[("ACTT", j)])
            post_proj(c, b, wp, lambda kc: b.ACTT[:, kc, :], lambda kc: ("ACTT", kc), NDFF,
                      lambda m: gains[:, g_post * KC + m:g_post * KC + m + 1], 0.5, src_dram, dst_dram, c.hT, t0)
    sch.barrier()


def win_groups():
    g = []
    for i in range(8): g.append(("cq", i, OFF_A + i * 128, 128))
    for i in range(4): g.append(("ckv", i, OFF_A + 1024 + i * 128, 128))
    g.append(("kpe", 0, OFF_A + 1536, 32)); g.append(("kpe", 1, OFF_A + 1568, 32))
    for i in range(8): g.append(("mqk", i, OFF_B + i * 128, 128))
    for i in range(8): g.append(("mv", i, OFF_B + 1024 + i * 128, 128))
    for i in range(8): g.append(("mo", i, OFF_B + 2048 + i * 128, 128))
    g.append(("mg", 0, OFF_B + 3072, 16))
    for i in range(8): g.append(("dq", i, OFF_C + i * 128, 128))
    for i in range(8): g.append(("dk", i, OFF_C + 1024 + i * 128, 128))
    for i in range(8): g.append(("dv", i, OFF_C + 2048 + i * 128, 128))
    for i in range(8): g.append(("sq", i, OFF_D + i * 128, 128))
    g.append(("sk", 0, OFF_D + 1024, 128)); g.append(("sv", 0, OFF_D + 1152, 128))
    return g


TOKMAJ = ("mv", "dv", "sv")


def evac(c, b, bank, rows, func, dst_ap, bf, res, bias=None, cols=TT):
    sch = c.sch
    if bf:
        o = rot(c, "OB", len(b.OB)); st = b.OB[o]; r = ("OB", o)
    else:
        o = rot(c, "OS", len(b.OS)); st = b.OS[o]; r = ("OS", o)
    kw = {} if bias is None else {"bias": bias}
    sch.op("act", (lambda e: e.activation(out=st[0:rows, 0:cols], in_=c.ps[bank][0:rows, 0:cols], func=func, **kw)),
           reads=[("ps", bank), "consts"], writes=[r])
    sch.op("sp", (lambda e: e.dma_start(out=dst_ap, in_=st[0:rows, 0:cols])), reads=[r], writes=[res], dma_key=r)


def proj_fm(c, b, w, rw, nk, rhs_fn, rhs_res_fn, bank, M):
    for kc in range(nk):
        c.sch.op("pe", (lambda e, kc=kc: e.matmul(c.ps[bank][0:M, :], lhsT=w[:, kc, 0:M], rhs=rhs_fn(kc), start=(kc == 0), stop=(kc == nk - 1))),
                 reads=[rw, rhs_res_fn(kc)], writes=[("ps", bank)])


def proj_tm(c, b, w, rw, nk, xn_fn, xn_res_fn, bank):
    for tb in range(4):
        for kc in range(nk):
            c.sch.op("pe", (lambda e, kc=kc, tb=tb: e.matmul(c.ps[bank][:, tb * P:(tb + 1) * P], lhsT=xn_fn(kc)[:, tb * P:(tb + 1) * P], rhs=w[:, kc, :],
                                                            start=(kc == 0), stop=(kc == nk - 1))),
                     reads=[rw, xn_res_fn(kc)], writes=[("ps", bank)])


def store_tm(c, b, bank, vdram, t0, col0, res):
    sch = c.sch
    o = rot(c, "OB", len(b.OB)); st = b.OB[o]; r = ("OB", o)
    sch.op("act", (lambda e: e.activation(out=st[:], in_=c.ps[bank][:], func=AF.Copy)), reads=[("ps", bank)], writes=[r])
    dst = vdram[t0:t0 + TT, col0:col0 + P].rearrange("(tb p) c -> p tb c", p=P)
    sch.op("sp", (lambda e: e.dma_start(out=dst, in_=st[:].rearrange("p (tb c) -> p tb c", c=P))), reads=[r], writes=[res], dma_key=r)


def rope(c, b, x1, r1, x2, r2, cs, dst, row0, t0, res):
    sch = c.sch
    cos, sin = cs
    def tmp():
        k = rot(c, "SG", len(b.SG)); return b.SG[k], ("SG", k)
    ta, ra = tmp(); tb_, rb = tmp()
    sch.op("dve", (lambda e: e.tensor_tensor(out=ta[0:32, :], in0=x1, in1=cos, op=ALU.mult)), reads=[r1, "cs"], writes=[ra])
    sch.op("dve", (lambda e: e.tensor_tensor(out=tb_[0:32, :], in0=x2, in1=sin, op=ALU.mult)), reads=[r2, "cs"], writes=[rb])
    o = rot(c, "OB", len(b.OB)); st = b.OB[o]; ro = ("OB", o)
    sch.op("dve", (lambda e: e.tensor_tensor(out=st[0:32, :], in0=ta[0:32, :], in1=tb_[0:32, :], op=ALU.subtract)), reads=[ra, rb], writes=[ro])
    sch.op("sp", (lambda e: e.dma_start(out=dst[row0:row0 + 32, t0:t0 + TT], in_=st[0:32, :])), reads=[ro], writes=[res + (0,)], dma_key=ro)
    tc_, rc = tmp(); td, rd = tmp()
    sch.op("dve", (lambda e: e.tensor_tensor(out=tc_[0:32, :], in0=x2, in1=cos, op=ALU.mult)), reads=[r2, "cs"], writes=[rc])
    sch.op("dve", (lambda e: e.tensor_tensor(out=td[0:32, :], in0=x1, in1=sin, op=ALU.mult)), reads=[r1, "cs"], writes=[rd])
    o2 = rot(c, "OB", len(b.OB)); st2 = b.OB[o2]; ro2 = ("OB", o2)
    sch.op("dve", (lambda e: e.tensor_tensor(out=st2[0:32, :], in0=tc_[0:32, :], in1=td[0:32, :], op=ALU.add)), reads=[rc, rd], writes=[ro2])
    sch.op("sp", (lambda e: e.dma_start(out=dst[row0 + 32:row0 + 64, t0:t0 + TT], in_=st2[0:32, :])), reads=[ro2], writes=[res + (1,)], dma_key=ro2)


def inproj_phase(c, l, src_dram, W):
    sch = c.sch
    T = c.T
    groups = win_groups()
    with ExitStack() as es:
        b = Bufs(c, es, wslot_elems=KC * P)
        XN2 = b.sb("XN2", [P, 8, TT], BF16)
        cos_t = b.sb("cos_t", [32, TT], F32)
        sin_t = b.sb("sin_t", [32, TT], F32)
        for tt in range(c.S // TT):
            t0 = tt * TT
            rms_rstd(c, b, src_dram, t0, KC, 7, 0)
            norm_to(c, b, src_dram, t0, KC, lambda kc: c.gains[:, 2 * KC + kc:2 * KC + kc + 1], 0,
                    lambda kc: b.XN[:, kc, :], lambda kc: ("XN", kc))
            sch.op("sp", lambda e, t0=t0: e.dma_start(out=cos_t[:], in_=c.cosT[:, t0:t0 + TT]), writes=["cs"], dma_key="cs0")
            sch.op("sp", lambda e, t0=t0: e.dma_start(out=sin_t[:], in_=c.sinT[:, t0:t0 + TT]), writes=["cs"], dma_key="cs")
            items = [("win", W["win"], gi, KC, P) for gi in range(len(groups))]
            items += [("uq", W["uq"], gi, 8, P) for gi in range(24)]
            items += [("ukv", W["ukv"], gi, 4, P) for gi in range(16)]
            wp = WPipe(c, b, items)
            xn_fn = lambda kc: b.XN[:, kc, :]
            xn_res = lambda kc: ("XN", kc)
            for gi, (nm, i, col0, wd_) in enumerate(groups):
                w, rw = wp.next()
                bank = gi % 4
                res = ("z", nm, i, tt)
                if nm in TOKMAJ:
                    proj_tm(c, b, w, rw, KC, xn_fn, xn_res, bank)
                    vd = {"mv": T["mV"], "dv": T["dV"], "sv": T["sV"]}[nm]
                    store_tm(c, b, bank, vd, t0, i * P, res)
                    continue
                proj_fm(c, b, w, rw, KC, xn_fn, xn_res, bank, wd_)
                if nm == "cq":
                    evac(c, b, bank, P, AF.Copy, T["cqT"][i * P:(i + 1) * P, t0:t0 + TT], False, res)
                elif nm == "ckv":
                    evac(c, b, bank, P, AF.Copy, T["ckvT"][i * P:(i + 1) * P, t0:t0 + TT], False, res)
                elif nm == "kpe":
                    evac(c, b, bank, 32, AF.Copy, T["kpeT"][i * 32:(i + 1) * 32, t0:t0 + TT], False, res)
                elif nm == "mqk":
                    evac(c, b, bank, P, AF.Copy, T["mqkT"][i * P:(i + 1) * P, t0:t0 + TT], False, res)
                elif nm == "mo":
                    evac(c, b, bank, P, AF.Sigmoid, T["moT"][i * P:(i + 1) * P, t0:t0 + TT], False, res)
                elif nm == "mg":
                    evac(c, b, bank, 16, AF.Copy, T["mgT"][0:16, t0:t0 + TT], False, res)
                elif nm == "dq":
                    evac(c, b, bank, P, AF.Copy, T["dqT"][i * P:(i + 1) * P, t0:t0 + TT], True, res)
                elif nm == "dk":
                    evac(c, b, bank, P, AF.Copy, T["dkT"][i * P:(i + 1) * P, t0:t0 + TT], True, res)
                elif nm == "sq":
                    evac(c, b, bank, P, AF.Copy, T["sqT"][i * P:(i + 1) * P, t0:t0 + TT], True, res)
                elif nm == "sk":
                    evac(c, b, bank, P, AF.Copy, T["skT"][0:P, t0:t0 + TT], True, res)
            cqres = lambda kc, tt=tt: ("z", "cq", kc, tt)
            rms_rstd(c, b, T["cqT"], t0, 8, 7, 0, res_fn=cqres)
            norm_to(c, b, T["cqT"], t0, 8, lambda kc: c.vec[:, c.V_QN + kc:c.V_QN + kc + 1], 0,
                    lambda kc: XN2[:, kc, :], lambda kc: ("XN2", kc), res_fn=cqres)
            x2_fn = lambda kc: XN2[:, kc, :]
            x2_res = lambda kc: ("XN2", kc)
            for h in range(8):
                w, rw = wp.next()
                proj_fm(c, b, w, rw, 8, x2_fn, x2_res, 0, P)
                evac(c, b, 0, P, AF.Copy, T["qnT"][h * P:(h + 1) * P, t0:t0 + TT], True, ("qn", h, tt))
                w, rw = wp.next()
                proj_fm(c, b, w, rw, 8, x2_fn, x2_res, 1, 32)
                w, rw = wp.next()
                proj_fm(c, b, w, rw, 8, x2_fn, x2_res, 2, 32)
                rope(c, b, c.ps[1][0:32, :], ("ps", 1), c.ps[2][0:32, :], ("ps", 2), (cos_t[:], sin_t[:]),
                     T["qrT"], h * 64, t0, ("qr", h, tt))
            ckres = lambda kc, tt=tt: ("z", "ckv", kc, tt)
            rms_rstd(c, b, T["ckvT"], t0, 4, 7, 0, res_fn=ckres)
            norm_to(c, b, T["ckvT"], t0, 4, lambda kc: c.vec[:, c.V_KVN + kc:c.V_KVN + kc + 1], 0,
                    lambda kc: XN2[:, kc, :], lambda kc: ("XN2", kc), res_fn=ckres)
            for h in range(8):
                w, rw = wp.next()
                proj_fm(c, b, w, rw, 4, x2_fn, x2_res, 0, P)
                evac(c, b, 0, P, AF.Copy, T["knT"][h * P:(h + 1) * P, t0:t0 + TT], True, ("kn", h, tt))
                w, rw = wp.next()
                proj_tm(c, b, w, rw, 4, x2_fn, x2_res, 3)
                store_tm(c, b, 3, T["aV"], t0, h * P, ("aV", h, tt))
            x1, rx1 = ld_tile(c, b, T["kpeT"][0:32, t0:t0 + TT], rows=32, reads=[("z", "kpe", 0, tt)])
            x2, rx2 = ld_tile(c, b, T["kpeT"][32:64, t0:t0 + TT], rows=32, reads=[("z", "kpe", 1, tt)])
            rope(c, b, x1[0:32, :], rx1, x2[0:32, :], rx2, (cos_t[:], sin_t[:]), T["krT"], 0, t0, ("kr", tt))
    sch.barrier()


def mlstm_prep(c, l):
    sch = c.sch
    T = c.T
    S = c.S
    with ExitStack() as es:
        b = Bufs(c, es, wslot_elems=64, nw=1, xn=False)
        CV = [b.sb("CV%d" % i, [P, TT + 4], F32) for i in range(2)]
        AC = [b.sb("AC%d" % i, [P, TT], F32) for i in range(2)]
        for cf in range(8):
            for tt in range(S // TT):
                t0 = tt * TT
                i = rot(c, "CV", 2); cv = CV[i]; rcv = ("CV", i)
                lo = max(t0 - 2, 0); hi = min(t0 + TT + 2, S)
                if t0 == 0:
                    sch.op("dve", (lambda e, cv=cv: e.memset(cv[:, 0:2], 0.0)), writes=[rcv])
                if t0 + TT == S:
                    sch.op("dve", (lambda e, cv=cv: e.memset(cv[:, TT + 2:TT + 4], 0.0)), writes=[rcv])
                sch.op("sp", (lambda e, cv=cv, lo=lo, hi=hi, t0=t0, cf=cf: e.dma_start(
                    out=cv[:, lo - (t0 - 2):hi - (t0 - 2)], in_=T["mqkT"][cf * P:(cf + 1) * P, lo:hi])),
                    writes=[rcv], dma_key=rcv)
                a0, a1 = AC[0], AC[1]
                wcol = lambda j, cf=cf: c.vec[:, c.V_CW + cf * 5 + j:c.V_CW + cf * 5 + j + 1]
                sch.op("dve", (lambda e, cv=cv, wc=wcol(0): e.tensor_scalar(out=a0[:], in0=cv[:, 0:TT], scalar1=wc, scalar2=None, op0=ALU.mult)),
                       reads=[rcv, "consts"], writes=["AC0"])
                cur, nxt, rc_, rn = a0, a1, "AC0", "AC1"
                for j in range(1, 5):
                    sch.op("dve", (lambda e, cv=cv, j=j, cur=cur, nxt=nxt, wc=wcol(j): e.scalar_tensor_tensor(
                        out=nxt[:], in0=cv[:, j:j + TT], scalar=wc, in1=cur[:], op0=ALU.mult, op1=ALU.add)),
                        reads=[rcv, rc_, "consts"], writes=[rn])
                    cur, nxt, rc_, rn = nxt, cur, rn, rc_
                o = rot(c, "OB", len(b.OB)); st = b.OB[o]; ro = ("OB", o)
                sch.op("act", (lambda e, cur=cur, st=st, cf=cf: e.activation(out=st[:], in_=cur[:], func=AF.Silu,
                                                                            bias=c.vec[:, c.V_CB + cf:c.V_CB + cf + 1])),
                       reads=[rc_, "consts"], writes=[ro])
                sch.op("sp", (lambda e, st=st, cf=cf, t0=t0: e.dma_start(out=T["mqkS"][cf * P:(cf + 1) * P, t0:t0 + TT], in_=st[:])),
                       reads=[ro], dma_key=ro)
        G = [b.sb("G%d" % i, [4, S], F32) for i in range(6)]
        gb = c.gateb

        def ldg(dst, ty):
            sch.op("sp", (lambda e: e.dma_start(out=dst[:], in_=T["mgT"][ty * 4:(ty + 1) * 4, :])), writes=[id(dst)], dma_key=("G", id(dst)))

        def scan(src, tmp, op, reverse):
            sh = 1
            cur, oth = src, tmp
            while sh < S:
                sch.op("act", (lambda e, cur=cur, oth=oth: e.activation(out=oth[:], in_=cur[:], func=AF.Copy)), reads=[id(cur)], writes=[id(oth)])
                if not reverse:
                    sch.op("dve", (lambda e, cur=cur, oth=oth, sh=sh: e.tensor_tensor(out=oth[:, sh:S], in0=cur[:, sh:S], in1=cur[:, 0:S - sh], op=op)),
                           reads=[id(cur)], writes=[id(oth)])
                else:
                    sch.op("dve", (lambda e, cur=cur, oth=oth, sh=sh: e.tensor_tensor(out=oth[:, 0:S - sh], in0=cur[:, 0:S - sh], in1=cur[:, sh:S], op=op)),
                           reads=[id(cur)], writes=[id(oth)])
                cur, oth = oth, cur
                sh *= 2
            return cur, oth

        def outrow(kind, d, src):
            sch.op("sp", (lambda e: e.dma_start(out=T["mrow"][kind, d], in_=src[:])), reads=[id(src)], dma_key=("Gout", kind, d))

        for d in range(2):
            li, lf, t2, t3, t4, t5 = G
            ldg(li, 2 * d); ldg(lf, 2 * d + 1)
            sch.op("dve", (lambda e, d=d: e.tensor_scalar(out=li[:], in0=li[:], scalar1=gb[:, 2 * d:2 * d + 1], scalar2=None, op0=ALU.add)),
                   reads=[id(li), "consts"], writes=[id(li)])
            sch.op("dve", (lambda e, d=d: e.tensor_scalar(out=lf[:], in0=lf[:], scalar1=gb[:, 2 * d + 1:2 * d + 2], scalar2=None, op0=ALU.add)),
                   reads=[id(lf), "consts"], writes=[id(lf)])
            sch.op("act", (lambda e: e.activation(out=lf[:], in_=lf[:], func=AF.Exp, scale=-1.0)), reads=[id(lf)], writes=[id(lf)])
            sch.op("dve", (lambda e: e.tensor_scalar(out=lf[:], in0=lf[:], scalar1=1.0, scalar2=None, op0=ALU.add)), reads=[id(lf)], writes=[id(lf)])
            sch.op("act", (lambda e: e.activation(out=lf[:], in_=lf[:], func=AF.Ln)), reads=[id(lf)], writes=[id(lf)])
            sch.op("dve", (lambda e: e.tensor_scalar(out=lf[:], in0=lf[:], scalar1=-1.0, scalar2=None, op0=ALU.mult)), reads=[id(lf)], writes=[id(lf)])
            sch.op("act", (lambda e: e.activation(out=t2[:], in_=lf[:], func=AF.Copy)), reads=[id(lf)], writes=[id(t2)])
            F, spare = scan(t2, t3, ALU.add, False)
            Bt = t4
            if d == 0:
                A = F
                sch.op("dve", (lambda e, Bt=Bt, li=li, F=F: e.tensor_tensor(out=Bt[:], in0=li[:], in1=F[:], op=ALU.subtract)), reads=[id(li), id(F)], writes=[id(Bt)])
            else:
                sch.op("dve", (lambda e, F=F, lf=lf: e.tensor_tensor(out=F[:], in0=F[:], in1=lf[:], op=ALU.subtract)), reads=[id(F), id(lf)], writes=[id(F)])
                sch.op("dve", (lambda e, Bt=Bt, li=li, F=F: e.tensor_tensor(out=Bt[:], in0=li[:], in1=F[:], op=ALU.add)), reads=[id(li), id(F)], writes=[id(Bt)])
            outrow(0, d, Bt)
            sch.op("act", (lambda e, t5=t5, Bt=Bt: e.activation(out=t5[:], in_=Bt[:], func=AF.Copy)), reads=[id(Bt)], writes=[id(t5)])
            M, sp2 = scan(t5, spare, ALU.max, d == 1)
            if d == 0:
                sch.op("dve", (lambda e, sp2=sp2, F=F, M=M: e.tensor_tensor(out=sp2[:], in0=F[:], in1=M[:], op=ALU.add)), reads=[id(F), id(M)], writes=[id(sp2)])
                sch.op("dve", (lambda e, sp2=sp2: e.tensor_scalar(out=sp2[:], in0=sp2[:], scalar1=-1.0, scalar2=None, op0=ALU.mult)), reads=[id(sp2)], writes=[id(sp2)])
            else:
                sch.op("dve", (lambda e, sp2=sp2, F=F, M=M: e.tensor_tensor(out=sp2[:], in0=F[:], in1=M[:], op=ALU.subtract)), reads=[id(F), id(M)], writes=[id(sp2)])
            outrow(2, d, sp2)
            sch.op("dve", (lambda e, M=M: e.tensor_scalar(out=M[:], in0=M[:], scalar1=-1.0, scalar2=None, op0=ALU.mult)), reads=[id(M)], writes=[id(M)])
            outrow(1, d, M)
    sch.barrier()


class ABufs:
    def __init__(self, c, es):
        nc = c.nc
        c.bufid += 1
        t = "a%d_" % c.bufid
        sb = lambda name, shape, dt: es.enter_context(nc.sbuf_tensor(t + name, shape, dt))
        self.sb = sb
        S = c.S
        self.KT = sb("KT", [P, 2, S], BF16)
        self.V = sb("V", [P, S // P, 256], BF16)
        self.Q = [sb("Q%d" % i, [P, 2, TT], BF16) for i in range(2)]
        self.E = [sb("E%d" % i, [P, TT], F32) for i in range(3)]
        self.ARG = [sb("ARG%d" % i, [P, TT], F32) for i in range(4)]
        self.PT = [sb("PT%d" % i, [P, TT], BF16) for i in range(6)]
        self.TMP = [sb("TMP%d" % i, [P, TT], F32) for i in range(6)]
        self.OS = [sb("OS%d" % i, [P, TT], F32) for i in range(3)]
        self.SQ = [sb("SQ%d" % i, [P, TT], BF16) for i in range(2)]
        self.HACC = sb("HACC", [P, 2, TT], F32)
        self.CONST = sb("CONST", [P, 19, TT], F32)
        self.BCOL = sb("BCOL", [P, S // P], F32)
        self.ROW = [sb("ROW%d" % i, [1, TT], F32) for i in range(4)]
        self.BC = sb("BC", [P, 4], F32)


def attn_tiles(c, A, tiles, qk_fn, nchunk, mode, scale, v_fn, nvh, M, qres, kres, vres):
    sch = c.sch
    n = len(tiles)
    pend = []
    banks = (0, 1) if mode == "mlstm" else (0, 1, 5, 6)
    depth = len(banks) - 1
    for ti, tl in enumerate(tiles):
        kt = tl["kt"]
        sb_ = banks[ti % len(banks)]
        for ch in range(nchunk):
            lhsT, rhs = qk_fn(kt, ch)
            sch.op("pe", (lambda e, lhsT=lhsT, rhs=rhs, ch=ch, sb_=sb_: e.matmul(c.ps[sb_][:], lhsT=lhsT, rhs=rhs, start=(ch == 0), stop=(ch == nchunk - 1))),
                   reads=[qres, kres], writes=[("ps", sb_)])
        pi = rot(c, "PT", len(A.PT)); pt = A.PT[pi]; rp = ("PT", pi)
        if mode == "plain":
            sch.op("act", (lambda e, pt=pt, sb_=sb_: e.activation(out=pt[:], in_=c.ps[sb_][:], func=AF.Exp, scale=scale)),
                   reads=[("ps", sb_)], writes=[rp])
        elif mode == "dist":
            ai = rot(c, "ARG", len(A.ARG)); ar = A.ARG[ai]; ra = ("ARG", ai)
            sch.op("dve", (lambda e, ar=ar, sb_=sb_, tl=tl: e.scalar_tensor_tensor(
                out=ar[:], in0=A.CONST[:, tl["ci"], :], scalar=float(tl["k1"]), in1=c.ps[sb_][:], op0=ALU.mult, op1=ALU.add)),
                reads=[("ps", sb_), "aconst"], writes=[ra])
            if tl["cc"] == 0.0:
                sch.op("act", (lambda e, pt=pt, ar=ar: e.activation(out=pt[:], in_=ar[:], func=AF.Exp, scale=scale)),
                       reads=[ra], writes=[rp])
            else:
                bi = rot(c, "BC", 4); rb_ = ("BC", bi)
                sch.op("dve", (lambda e, bi=bi, tl=tl: e.memset(A.BC[:, bi:bi + 1], float(tl["cc"]))), writes=[rb_])
                sch.op("act", (lambda e, pt=pt, ar=ar, bi=bi: e.activation(out=pt[:], in_=ar[:], func=AF.Exp, scale=scale, bias=A.BC[:, bi:bi + 1])),
                       reads=[ra, rb_], writes=[rp])
        else:
            ei = rot(c, "E", len(A.E)); ee = A.E[ei]; re_ = ("E", ei)
            ai = rot(c, "ARG", len(A.ARG)); ar = A.ARG[ai]; ra = ("ARG", ai)
            if tl.get("mask") is not None:
                sch.op("dve", (lambda e, ar=ar, tl=tl, kt=kt: e.scalar_tensor_tensor(out=ar[:], in0=c.ps[5][:], scalar=A.BCOL[:, kt:kt + 1],
                                                                                in1=A.CONST[:, tl["mask"], :], op0=ALU.add, op1=ALU.add)),
                       reads=[("ps", 5), "aconst", "bcol"], writes=[ra])
            else:
                sch.op("dve", (lambda e, ar=ar, kt=kt: e.tensor_scalar(out=ar[:], in0=c.ps[5][:], scalar1=A.BCOL[:, kt:kt + 1], scalar2=None, op0=ALU.add)),
                       reads=[("ps", 5), "bcol"], writes=[ra])
            sch.op("act", (lambda e, ee=ee, ar=ar: e.activation(out=ee[:], in_=ar[:], func=AF.Exp)), reads=[ra], writes=[re_])
            sch.op("dve", (lambda e, pt=pt, ee=ee, sb_=sb_: e.scalar_tensor_tensor(
                out=pt[:], in0=ee[:], scalar=float(scale), in1=c.ps[sb_][:], op0=ALU.mult, op1=ALU.mult)),
                reads=[re_, ("ps", sb_)], writes=[rp])

        def mk(pt=pt, rp=rp, kt=kt, ti=ti):
            def f():
                for hv in range(nvh):
                    sch.op("pe", (lambda e, hv=hv: e.matmul(c.ps[2 + hv][0:M, :], lhsT=v_fn(kt, hv), rhs=pt[:], start=(ti == 0), stop=(ti == n - 1))),
                           reads=[rp, vres], writes=[("ps", 2 + hv)])
                sch.op("pe", (lambda e: e.matmul(c.ps[4][:], lhsT=c.ones_bf[:], rhs=pt[:], start=(ti == 0), stop=(ti == n - 1))),
                       reads=[rp, "ones_bf"], writes=[("ps", 4)])
            return f
        pend.append(mk())
        if len(pend) > depth:
            pend.pop(0)()
    while pend:
        pend.pop(0)()


def load_k(c, A, slot, src_ap, rows, base, reads=()):
    c.sch.op("sp", (lambda e: e.dma_start(out=A.KT[base:base + rows, slot, :], in_=src_ap)), reads=list(reads), writes=["KT"], dma_key=("KT", slot, base))


def load_v(c, A, vdram, col0, dv):
    src = vdram[:, col0:col0 + dv].rearrange("(kt p) c -> p kt c", p=P)
    c.sch.op("sp", (lambda e: e.dma_start(out=A.V[:, :, 0:dv], in_=src)), writes=["V"], dma_key="V")


def load_q(c, A, slot_list):
    i = rot(c, "Q", 2); q = A.Q[i]; r = ("Q", i)
    for (sl, src, rows, base) in slot_list:
        c.sch.op("sp", (lambda e, sl=sl, src=src, rows=rows, base=base: e.dma_start(out=q[base:base + rows, sl, :], in_=src)),
                 writes=[r], dma_key=("Q", i, sl, base))
    return q, r


def tmp(c, A):
    k = rot(c, "TMP", len(A.TMP)); return A.TMP[k], ("TMP", k)


def store_out(c, A, src_fn, rows, dst_ap):
    o = rot(c, "AOS", len(A.OS)); st = A.OS[o]; r = ("AOS", o)
    src_fn(st, r)
    c.sch.op("sp", (lambda e: e.dma_start(out=dst_ap, in_=st[0:rows, :])), reads=[r], dma_key=r)


def mla_attn(c, A):
    sch = c.sch; T = c.T; S = c.S
    nkt = S // P
    scale = (128 + 64) ** -0.5
    for h in range(8):
        load_k(c, A, 0, T["knT"][h * P:(h + 1) * P, :], P, 0)
        load_k(c, A, 1, T["krT"][0:64, :], 64, 0)
        load_v(c, A, T["aV"], h * P, P)
        for qt in range(S // TT):
            t0 = qt * TT
            q, rq = load_q(c, A, [(0, T["qnT"][h * P:(h + 1) * P, t0:t0 + TT], P, 0), (1, T["qrT"][h * 64:(h + 1) * 64, t0:t0 + TT], 64, 0)])
            def qk(kt, ch, q=q):
                if ch == 0:
                    return A.KT[:, 0, kt * P:(kt + 1) * P], q[:, 0, :]
                return A.KT[0:64, 1, kt * P:(kt + 1) * P], q[0:64, 1, :]
            attn_tiles(c, A, [{"kt": kt} for kt in range(nkt)], qk, 2, "plain", scale,
                       lambda kt, hv: A.V[:, kt, 0:P], 1, P, rq, "KT", "V")
            rz, rrz = tmp(c, A)
            sch.op("dve", (lambda e, rz=rz: e.reciprocal(out=rz[:], in_=c.ps[4][:])), reads=[("ps", 4)], writes=[rrz])
            def fin(st, r, rz=rz, rrz=rrz):
                sch.op("dve", (lambda e: e.tensor_tensor(out=st[:], in0=c.ps[2][:], in1=rz[:], op=ALU.mult)), reads=[("ps", 2), rrz], writes=[r])
            store_out(c, A, fin, P, T["yT"][h * P:(h + 1) * P, t0:t0 + TT])


def diff_attn(c, A):
    sch = c.sch; T = c.T; S = c.S
    nkt = S // P
    scale = 64 ** -0.5
    for h in range(8):
        slope = 2.0 ** (-(h + 1))
        load_k(c, A, 0, T["dkT"][h * P:(h + 1) * P, :], P, 0)
        load_v(c, A, T["dV"], h * P, P)
        for qt in range(S // TT):
            t0 = qt * TT
            q, rq = load_q(c, A, [(0, T["dqT"][h * P:(h + 1) * P, t0:t0 + TT], P, 0)])
            tiles = []
            for kt in range(nkt):
                k0 = kt * P
                r = (k0 - t0) // P
                if 0 <= r <= 3:
                    tiles.append({"kt": kt, "ci": 1 + r, "k1": -slope / scale, "cc": 0.0})
                elif k0 < t0:
                    if slope * (t0 - k0 - 127) > 120.0:
                        continue
                    tiles.append({"kt": kt, "ci": 0, "k1": -slope / scale, "cc": -slope * (t0 - k0)})
                else:
                    if slope * (k0 - t0 - 511) > 120.0:
                        continue
                    tiles.append({"kt": kt, "ci": 0, "k1": slope / scale, "cc": slope * (t0 - k0)})
            d1, rd1 = tmp(c, A)
            for mp in range(2):
                base = 64 * mp
                def qk(kt, ch, q=q, base=base):
                    return A.KT[base:base + 64, 0, kt * P:(kt + 1) * P], q[base:base + 64, 0, :]
                attn_tiles(c, A, tiles, qk, 1, "dist", scale, lambda kt, hv: A.V[:, kt, 0:P], 1, P, rq, "KT", "V")
                rz, rrz = tmp(c, A)
                sch.op("dve", (lambda e, rz=rz: e.reciprocal(out=rz[:], in_=c.ps[4][:])), reads=[("ps", 4)], writes=[rrz])
                if mp == 0:
                    sch.op("dve", (lambda e, rz=rz, d1=d1: e.tensor_tensor(out=d1[:], in0=c.ps[2][:], in1=rz[:], op=ALU.mult)), reads=[("ps", 2), rrz], writes=[rd1])
                else:
                    d2, rd2 = tmp(c, A)
                    sch.op("dve", (lambda e, rz=rz, d2=d2: e.tensor_tensor(out=d2[:], in0=c.ps[2][:], in1=rz[:], op=ALU.mult)), reads=[("ps", 2), rrz], writes=[rd2])
                    sch.op("dve", (lambda e, d2=d2, d1=d1: e.scalar_tensor_tensor(out=d1[:], in0=d2[:], scalar=c.vec[:, c.V_NLAM:c.V_NLAM + 1], in1=d1[:], op0=ALU.mult, op1=ALU.add)),
                           reads=[rd1, rd2, "consts"], writes=[rd1])
            j = rot(c, "ASQ", 2); sq = A.SQ[j]; rs = ("ASQ", j)
            sch.op("act", (lambda e, sq=sq, d1=d1: e.activation(out=sq[:], in_=d1[:], func=AF.Square)), reads=[rd1], writes=[rs])
            sch.op("pe", (lambda e, sq=sq: e.matmul(c.ps[7][:], lhsT=c.ones_bf[:], rhs=sq[:], start=True, stop=True)), reads=[rs, "ones_bf"], writes=[("ps", 7)])
            t1, rt1 = tmp(c, A)
            sch.op("act", (lambda e, t1=t1: e.activation(out=t1[:], in_=c.ps[7][:], func=AF.Sqrt, bias=c.epsc[:, 0:1], scale=1.0 / P)), reads=[("ps", 7)], writes=[rt1])
            sch.op("dve", (lambda e, t1=t1: e.reciprocal(out=t1[:], in_=t1[:])), reads=[rt1], writes=[rt1])
            def fin(st, r, d1=d1, rd1=rd1, t1=t1, rt1=rt1):
                sch.op("dve", (lambda e: e.scalar_tensor_tensor(out=st[:], in0=d1[:], scalar=c.vec[:, c.V_SUBLN:c.V_SUBLN + 1], in1=t1[:], op0=ALU.mult, op1=ALU.mult)),
                       reads=[rd1, rt1, "consts"], writes=[r])
            store_out(c, A, fin, P, T["yT"][2048 + h * P:2048 + (h + 1) * P, t0:t0 + TT])


def swa_attn(c, A):
    sch = c.sch; T = c.T; S = c.S
    nkt = S // P
    scale = 64 ** -0.5
    for g in range(2):
        load_k(c, A, 0, T["skT"][g * 64:(g + 1) * 64, :], 64, 0)
        load_k(c, A, 0, T["skT"][g * 64:(g + 1) * 64, :], 64, 64)
        load_v(c, A, T["sV"], g * 64, 64)
        for r8 in range(8):
            hh = g * 8 + r8
            slope = 2.0 ** (-(hh + 1) / 2.0)
            base = 64 * (hh % 2)
            for qt in range(S // TT):
                t0 = qt * TT
                q, rq = load_q(c, A, [(0, T["sqT"][(hh // 2) * P:(hh // 2 + 1) * P, t0:t0 + TT], P, 0)])
                tiles = []
                for rr in range(6):
                    kt = 4 * qt - 1 + rr
                    if 0 <= kt < nkt:
                        tiles.append({"kt": kt, "ci": 5 + rr, "k1": -slope / scale, "cc": 0.0})
                def qk(kt, ch, q=q, base=base):
                    return A.KT[base:base + 64, 0, kt * P:(kt + 1) * P], q[base:base + 64, 0, :]
                attn_tiles(c, A, tiles, qk, 1, "dist", scale, lambda kt, hv: A.V[:, kt, 0:64], 1, 64, rq, "KT", "V")
                rz, rrz = tmp(c, A)
                sch.op("dve", (lambda e, rz=rz, hh=hh: e.tensor_scalar(out=rz[:], in0=c.ps[4][:], scalar1=c.vec[:, c.V_SINK + hh:c.V_SINK + hh + 1], scalar2=None, op0=ALU.add)),
                       reads=[("ps", 4), "consts"], writes=[rrz])
                sch.op("dve", (lambda e, rz=rz: e.reciprocal(out=rz[:], in_=rz[:])), reads=[rrz], writes=[rrz])
                def fin(st, r, rz=rz, rrz=rrz):
                    sch.op("dve", (lambda e: e.tensor_tensor(out=st[0:64, :], in0=c.ps[2][0:64, :], in1=rz[0:64, :], op=ALU.mult)), reads=[("ps", 2), rrz], writes=[r])
                store_out(c, A, fin, 64, T["yT"][3072 + hh * 64:3072 + (hh + 1) * 64, t0:t0 + TT])


def mlstm_attn(c, A):
    sch = c.sch; T = c.T; S = c.S
    nkt = S // P
    scale = 128 ** -0.5
    for h in range(4):
        load_k(c, A, 0, T["mqkS"][512 + h * P:512 + (h + 1) * P, :], P, 0)
        load_v(c, A, T["mV"], h * 256, 256)
        for d in range(2):
            sch.op("sp", (lambda e, d=d, h=h: e.dma_start(out=A.BCOL[:], in_=T["mrow"][0, d, h].rearrange("(kt p) -> p kt", p=P), allow_slow_non_contiguous=True)),
                   writes=["bcol"], dma_key="bcol")
            for qt in range(S // TT):
                t0 = qt * TT
                q, rq = load_q(c, A, [(0, T["mqkS"][h * P:(h + 1) * P, t0:t0 + TT], P, 0)])
                for kind, bank in ((1, 5), (2, 6)):
                    ri = rot(c, "ROW", len(A.ROW)); row = A.ROW[ri]; rr_ = ("ROW", ri)
                    sch.op("sp", (lambda e, row=row, kind=kind, d=d, h=h, t0=t0: e.dma_start(out=row[:], in_=T["mrow"][kind, d, h:h + 1, t0:t0 + TT])),
                           writes=[rr_], dma_key=rr_)
                    sch.op("pe", (lambda e, row=row, bank=bank: e.matmul(c.ps[bank][:], lhsT=c.ones_row[:], rhs=row[:], start=True, stop=True)),
                           reads=[rr_, "ones_bf"], writes=[("ps", bank)])
                tiles = []
                for kt in range(nkt):
                    r = kt - 4 * qt
                    if 0 <= r <= 3:
                        tiles.append({"kt": kt, "mask": (11 + r) if d == 0 else (15 + r)})
                    elif (r < 0 and d == 0) or (r > 3 and d == 1):
                        tiles.append({"kt": kt, "mask": None})
                def qk(kt, ch, q=q):
                    return A.KT[:, 0, kt * P:(kt + 1) * P], q[:, 0, :]
                attn_tiles(c, A, tiles, qk, 1, "mlstm", scale, lambda kt, hv: A.V[:, kt, hv * P:(hv + 1) * P], 2, P, rq, "KT", "V")
                da, rda = tmp(c, A)
                sch.op("dve", (lambda e, da=da: e.tensor_scalar(out=da[:], in0=c.ps[4][:], scalar1=-1.0, scalar2=None, op0=ALU.mult)), reads=[("ps", 4)], writes=[rda])
                sch.op("dve", (lambda e, da=da: e.tensor_tensor(out=da[:], in0=da[:], in1=c.ps[4][:], op=ALU.max)), reads=[("ps", 4), rda], writes=[rda])
                en, ren = tmp(c, A)
                sch.op("act", (lambda e, en=en: e.activation(out=en[:], in_=c.ps[6][:], func=AF.Exp)), reads=[("ps", 6)], writes=[ren])
                sch.op("dve", (lambda e, da=da, en=en: e.tensor_tensor(out=da[:], in0=da[:], in1=en[:], op=ALU.max)), reads=[rda, ren], writes=[rda])
                sch.op("dve", (lambda e, da=da: e.reciprocal(out=da[:], in_=da[:])), reads=[rda], writes=[rda])
                for hv in range(2):
                    if d == 0:
                        sch.op("dve", (lambda e, hv=hv, da=da: e.tensor_tensor(out=A.HACC[:, hv, :], in0=c.ps[2 + hv][:], in1=da[:], op=ALU.mult)),
                               reads=[("ps", 2 + hv), rda], writes=[("HACC", hv, qt)])
                        hst, rhst = tmp(c, A)
                        sch.op("act", (lambda e, hv=hv, hst=hst: e.activation(out=hst[:], in_=A.HACC[:, hv, :], func=AF.Copy)), reads=[("HACC", hv, qt)], writes=[rhst])
                        sch.op("sp", (lambda e, hv=hv, hst=hst, h=h, t0=t0: e.dma_start(out=T["mhf"][h * 256 + hv * P:h * 256 + (hv + 1) * P, t0:t0 + TT], in_=hst[:])),
                               reads=[rhst], writes=[("mhf", h, hv, qt)], dma_key=rhst)
                    else:
                        hb, rhb = tmp(c, A)
                        sch.op("dve", (lambda e, hv=hv, da=da, hb=hb: e.tensor_tensor(out=hb[:], in0=c.ps[2 + hv][:], in1=da[:], op=ALU.mult)),
                               reads=[("ps", 2 + hv), rda], writes=[rhb])
                        hf, rhf = tmp(c, A)
                        sch.op("sp", (lambda e, hv=hv, hf=hf, h=h, t0=t0: e.dma_start(out=hf[:], in_=T["mhf"][h * 256 + hv * P:h * 256 + (hv + 1) * P, t0:t0 + TT])),
                               reads=[("mhf", h, hv, qt)], writes=[rhf], dma_key=rhf)
                        og, rog = tmp(c, A)
                        sch.op("sp", (lambda e, hv=hv, og=og, h=h, t0=t0: e.dma_start(out=og[:], in_=T["moT"][h * 256 + hv * P:h * 256 + (hv + 1) * P, t0:t0 + TT])),
                               writes=[rog], dma_key=rog)
                        sch.op("dve", (lambda e, hb=hb, hf=hf: e.tensor_tensor(out=hb[:], in0=hb[:], in1=hf[:], op=ALU.add)), reads=[rhb, rhf], writes=[rhb])
                        def fin(st, r, hb=hb, rhb=rhb, og=og, rog=rog):
                            sch.op("dve", (lambda e: e.tensor_tensor(out=st[:], in0=hb[:], in1=og[:], op=ALU.mult)), reads=[rhb, rog], writes=[r])
                        store_out(c, A, fin, P, T["yT"][1024 + h * 256 + hv * P:1024 + h * 256 + (hv + 1) * P, t0:t0 + TT])


def attn_phase(c, l):
    sch = c.sch
    with ExitStack() as es:
        A = ABufs(c, es)
        sch.op("sp", lambda e: e.dma_start(out=A.CONST[:], in_=c.aconst), writes=["aconst"], dma_key="aconst")
        mla_attn(c, A)
        mlstm_attn(c, A)
        diff_attn(c, A)
        swa_attn(c, A)
    sch.barrier()


def out_phase(c, l, src_dram, dst_dram, wout):
    sch = c.sch; T = c.T
    with ExitStack() as es:
        b = Bufs(c, es, wslot_elems=KC * P)
        for tt in range(c.S // TT):
            t0 = tt * TT
            for gi in range(4):
                rms_rstd(c, b, T["yT"][gi * 1024:(gi + 1) * 1024, :], t0, 8, 7, 0)
                norm_to(c, b, T["yT"][gi * 1024:(gi + 1) * 1024, :], t0, 8,
                        lambda kc, gi=gi: c.vec[:, c.V_GN + gi * 8 + kc:c.V_GN + gi * 8 + kc + 1], 0,
                        lambda kc, gi=gi: b.XN[:, gi * 8 + kc, :], lambda kc, gi=gi: ("XN", gi * 8 + kc))
            wp = WPipe(c, b, [("wout", wout, m, KC, P) for m in range(KC)])
            post_proj(c, b, wp, lambda kc: b.XN[:, kc, :], lambda kc: ("XN", kc), KC,
                      lambda m: c.gains[:, 3 * KC + m:3 * KC + m + 1], 1.0, src_dram, dst_dram, c.hT, t0)
    sch.barrier()


NVIN = 367
NV = 385
WSPEC = [("win", 73, KC * P), ("uq", 24, 8 * P), ("ukv", 16, 4 * P), ("wout", KC, KC * P),
         ("f1gu", 2 * NDFF, KC * P), ("f1dn", KC, NDFF * P), ("f2gu", 2 * NDFF, KC * P), ("f2dn", KC, NDFF * P)]


def build(S, L):
    nc = bass.Bass("TRN2", target_bir_lowering=False)
    c = Ctx()
    c.nc = nc; c.S = S; c.sch = Sched(nc); c.rot = {}; c.bufid = 0
    sch = c.sch
    din = lambda name, shape, dt=F32: nc.dram_tensor(name, shape, dt, kind="ExternalInput").ap()
    dsc = lambda name, shape, dt: nc.dram_tensor(name, shape, dt).ap()
    xT = din("xT", [D, S])
    yout = nc.dram_tensor("yT_out", [D, S], F32, kind="ExternalOutput").ap()
    gains_in = din("gains", [L, P, 6 * KC])
    vec_in = din("vec", [L, P, NVIN])
    gateb_in = din("gateb", [L, 4, 4])
    c.cosT = din("cosT", [32, S]); c.sinT = din("sinT", [32, S])
    c.aconst = din("aconst", [P, 19, TT])
    Win = {n: din(n, [L, ng, P, row]) for (n, ng, row) in WSPEC}
    npar = 2 if L > 1 else 1
    Wbf2 = [{n: dsc(n + "_bf%d" % par, [ng, P, row], BF16) for (n, ng, row) in WSPEC} for par in range(npar)]

    def cast_layer(l):
        par = l % npar
        for (n, ng, row) in WSPEC:
            cast_weight(c, n + str(par), Win[n][l], Wbf2[par][n], ng, row)
    T = {}
    for n, rows, dt in [("cqT", 1024, F32), ("ckvT", 512, F32), ("kpeT", 64, F32), ("mqkT", 1024, F32), ("moT", 1024, F32),
                        ("mgT", 16, F32), ("dqT", 1024, BF16), ("dkT", 1024, BF16), ("sqT", 1024, BF16), ("skT", 128, BF16),
                        ("qnT", 1024, BF16), ("qrT", 512, BF16), ("knT", 1024, BF16), ("krT", 64, BF16), ("mqkS", 1024, BF16),
                        ("yT", 4096, F32), ("mhf", 1024, F32)]:
        if Ctx.debug and n in ("yT", "mqkS", "moT", "mhf", "mqkT", "mgT"):
            T[n] = nc.dram_tensor("t_" + n, [rows, S], dt, kind="ExternalOutput").ap()
        else:
            T[n] = dsc("t_" + n, [rows, S], dt)
    for n, cols in [("mV", 1024), ("dV", 1024), ("sV", 128), ("aV", 1024)]:
        if Ctx.debug and n == "mV":
            T[n] = nc.dram_tensor("t_" + n, [S, cols], BF16, kind="ExternalOutput").ap()
        else:
            T[n] = dsc("t_" + n, [S, cols], BF16)
    if Ctx.debug:
        T["mrow"] = nc.dram_tensor("t_mrow", [3, 2, 4, S], F32, kind="ExternalOutput").ap()
    else:
        T["mrow"] = dsc("t_mrow", [3, 2, 4, S], F32)
    c.T = T
    c.hT = dsc("hT", [D, TT], F32)
    xA = dsc("xA", [D, S], F32); xB = dsc("xB", [D, S], F32); xC = dsc("xC", [D, S], F32)
    c.V_QN, c.V_KVN, c.V_CW, c.V_CB, c.V_GN = 0, 8, 12, 52, 60
    V_SUBRAW, V_LAMINIT, V_OML, V_SINKRAW, V_LAMVEC = 92, 93, 94, 95, 111
    c.V_SUBLN, c.V_NLAM, c.V_SINK = 367, 368, 369
    with ExitStack() as es:
        c.ps = [es.enter_context(nc.psum_tensor("ps%d" % i, [P, 512], F32)) for i in range(8)]
        sbg = lambda name, shape, dt: es.enter_context(nc.sbuf_tensor(name, shape, dt))
        c.ones_bf = sbg("ones_bf", [P, P], BF16)
        c.ones_row = sbg("ones_row", [1, P], F32)
        c.gains = sbg("gains_sb", [P, 6 * KC], F32)
        c.vec = sbg("vec_sb", [P, NV], F32)
        c.gateb = sbg("gateb_sb", [4, 4], F32)
        vt = sbg("vtmp", [P, 64], F32)
        c.epsc = sbg("epsc", [P, 1], F32)
        sch.op("dve", lambda e: e.memset(c.epsc[:], EPS), writes=["ones_bf"])
        vs = sbg("vsum", [P, 4], F32)
        sch.op("dve", lambda e: e.memset(c.ones_bf[:], 1.0), writes=["ones_bf"])
        sch.op("dve", lambda e: e.memset(c.ones_row[:], 1.0), writes=["ones_bf"])
        cur = xT
        for l in range(L):
            if l == 0:
                cast_layer(0)
            if l + 1 < L:
                cast_layer(l + 1)
            Wbf = Wbf2[l % npar]
            c.wsuf = str(l % npar)
            sch.op("sp", lambda e, l=l: e.dma_start(out=c.gains[:], in_=gains_in[l]), writes=["consts"], dma_key="cg")
            sch.op("sp", lambda e, l=l: e.dma_start(out=c.vec[:, 0:NVIN], in_=vec_in[l]), writes=["consts"], dma_key="cv")
            sch.op("sp", lambda e, l=l: e.dma_start(out=c.gateb[:], in_=gateb_in[l]), writes=["consts"], dma_key="cgb")
            v = c.vec
            sch.op("dve", lambda e: e.tensor_tensor(out=v[:, c.V_SUBLN:c.V_SUBLN + 1], in0=v[:, V_SUBRAW:V_SUBRAW + 1], in1=v[:, V_OML:V_OML + 1], op=ALU.mult),
                   reads=["consts"], writes=["consts"])
            for k in range(2):
                sch.op("dve", lambda e, k=k: e.tensor_tensor(out=vt[:], in0=v[:, V_LAMVEC + 128 * k:V_LAMVEC + 128 * k + 64],
                                                              in1=v[:, V_LAMVEC + 128 * k + 64:V_LAMVEC + 128 * k + 128], op=ALU.mult),
                       reads=["consts"], writes=["vt"])
                sch.op("dve", lambda e, k=k: e.reduce_sum(out=vs[:, k:k + 1], in_=vt[:], axis=mybir.AxisListType.X), reads=["vt"], writes=["vs"])
            sch.op("act", lambda e: e.activation(out=vs[:, 0:2], in_=vs[:, 0:2], func=AF.Exp), reads=["vs"], writes=["vs"])
            sch.op("dve", lambda e: e.tensor_tensor(out=vs[:, 2:3], in0=vs[:, 1:2], in1=vs[:, 0:1], op=ALU.subtract), reads=["vs"], writes=["vs"])
            sch.op("dve", lambda e: e.tensor_tensor(out=v[:, c.V_NLAM:c.V_NLAM + 1], in0=vs[:, 2:3], in1=v[:, V_LAMINIT:V_LAMINIT + 1], op=ALU.subtract),
                   reads=["vs", "consts"], writes=["consts"])
            sch.op("act", lambda e: e.activation(out=v[:, c.V_SINK:c.V_SINK + 16], in_=v[:, V_SINKRAW:V_SINKRAW + 16], func=AF.Exp),
                   reads=["consts"], writes=["consts"])
            last = (l == L - 1)
            ffn_phase(c, cur, (xA if c.stages >= 2 else yout), Wbf["f1gu"], Wbf["f1dn"], c.gains, 0, 1, "f1")
            if c.stages >= 2:
                inproj_phase(c, l, xA, Wbf)
                mlstm_prep(c, l)
                attn_phase(c, l)
                out_phase(c, l, xA, xB, Wbf["wout"])
                ffn_phase(c, xB, (yout if last else xC), Wbf["f2gu"], Wbf["f2dn"], c.gains, 4, 5, "f2")
            cur = xC
        c.n_dma_keys = len(sch.dma_keys)
        sch.emit()
    return nc, c


Ctx.stages = 2
Ctx.debug = False


def relayout(W, cols_list):
    K = W.shape[0]
    out = np.zeros((len(cols_list), P, K // P, P), np.float32)
    for g, (c0, w) in enumerate(cols_list):
        out[g, :, :, :w] = W[:, c0:c0 + w].reshape(K // P, P, w).transpose(1, 0, 2)
    return out.reshape(len(cols_list), P, -1)


def relayout_uniform(W):
    K, N = W.shape
    return np.ascontiguousarray(W.reshape(K // P, P, N // P, P).transpose(2, 1, 0, 3)).reshape(N // P, P, -1)


def layer_inputs(inp, l):
    o = {}
    o["win"] = relayout(inp["w_in"][l], [(c0, w) for (_, _, c0, w) in win_groups()])
    uqc = []
    for h in range(8):
        uqc += [(h * 192, 128), (h * 192 + 128, 32), (h * 192 + 160, 32)]
    o["uq"] = relayout(inp["mla_w_uq"][l], uqc)
    kvc = []
    for h in range(8):
        kvc += [(h * 256, 128), (h * 256 + 128, 128)]
    o["ukv"] = relayout(inp["mla_w_ukv"][l], kvc)
    o["wout"] = relayout_uniform(inp["w_out"][l])
    o["f1gu"] = relayout_uniform(inp["ffn1_w_gu"][l]); o["f1dn"] = relayout_uniform(inp["ffn1_w_down"][l])
    o["f2gu"] = relayout_uniform(inp["ffn2_w_gu"][l]); o["f2dn"] = relayout_uniform(inp["ffn2_w_down"][l])
    o["gains"] = np.ascontiguousarray(inp["norm_gains"][l].reshape(6, KC, P).transpose(2, 0, 1)).reshape(P, 6 * KC)
    vec = np.zeros((P, NVIN), np.float32)
    vec[:, 0:8] = inp["mla_q_norm"][l].reshape(8, P).T
    vec[:, 8:12] = inp["mla_kv_norm"][l].reshape(4, P).T
    vec[:, 12:52] = inp["mlstm_conv_w"][l].reshape(5, 8, P).transpose(2, 1, 0).reshape(P, 40)
    vec[:, 52:60] = inp["mlstm_conv_b"][l].reshape(8, P).T
    vec[:, 60:92] = inp["group_norm"][l].reshape(32, P).T
    vec[:, 92] = inp["diff_subln"][l]
    lam_init = 0.8 - 0.6 * math.exp(-0.3 * l)
    vec[:, 93] = lam_init
    vec[:, 94] = 1.0 - lam_init
    vec[:, 95:111] = inp["swa_sink"][l][None, :]
    vec[:, 111:367] = inp["diff_lambda"][l].reshape(1, 256)
    o["vec"] = vec
    o["gateb"] = np.ascontiguousarray(inp["mlstm_gate_b"][l].T)
    return o


def const_inputs(S):
    inv = 10000.0 ** (-np.arange(32, dtype=np.float32) / 32)
    ang = np.arange(S, dtype=np.float32)[None, :] * inv[:, None]
    k = np.arange(P, dtype=np.float32)[:, None]
    q = np.arange(TT, dtype=np.float32)[None, :]
    ac = np.zeros((P, 19, TT), np.float32)
    ac[:, 0] = q - k
    for r in range(4):
        ac[:, 1 + r] = np.abs(q - k - 128 * r)
        ac[:, 11 + r] = np.where(128 * r + k <= q, 0.0, NEG)
        ac[:, 15 + r] = np.where(128 * r + k >= q, 0.0, NEG)
    for rr in range(6):
        dist = np.abs(q - (rr - 1) * 128 - k)
        ac[:, 5 + rr] = np.where(dist <= 128, dist, 1.0e6)
    return {"cosT": np.cos(ang).astype(np.float32), "sinT": np.sin(ang).astype(np.float32), "aconst": ac}


_PROG = {}


def kernel(**inp):
    inp = {k: np.asarray(v) for k, v in inp.items()}
    x = inp["x"]
    B, S, _ = x.shape
    L = inp["w_in"].shape[0]
    key = (S, L)
    if key not in _PROG:
        _PROG[key] = build(S, L)[0]
    nc = _PROG[key]
    shared = const_inputs(S)
    per_layer = [layer_inputs(inp, l) for l in range(L)]
    for k in per_layer[0]:
        shared[k] = np.stack([pl[k] for pl in per_layer], axis=0)
    maps = []
    for b in range(B):
        m = dict(shared)
        m["xT"] = np.ascontiguousarray(x[b].T)
        maps.append(m)
    res = run_bass_kernel_spmd(nc, maps, core_ids=list(range(B)))
    return np.stack([np.asarray(res.results[b]["yT_out"]).T for b in range(B)], axis=0).astype(np.float32)
```
